# Optimizing a Trainium2 kernel written in Bass

```python
import math
import jax, jax.numpy as jnp
from jax import lax
import numpy as np

D_MODEL = 1024
BATCH = 16
SEQ = 2048
DEPTH = 4

MLA_HEADS = 8
MLA_NOPE = 64
MLA_ROPE = 32
MLA_V = 64
MLA_Q_LORA = 384
MLA_KV_LORA = 256
ROPE_BASE = 10000.0
DIFF_HEADS = 8
DIFF_HD = 64
DIFF_VD = 2 * DIFF_HD
D_FF = 2816
N_EXPERTS = 8
TOP_K = 2
N_DENSE = (DEPTH + 1) // 2
N_MOE = DEPTH // 2
Q_BLOCK = 128
EXPERT_BLOCK = 128
DEEPNORM_ALPHA = (2.0 * DEPTH) ** 0.25
DEEPNORM_BETA = (8.0 * DEPTH) ** -0.25
LN_EPS = 1e-5
RMS_EPS = 1e-6
SPLIT_SIZES = (MLA_Q_LORA, MLA_KV_LORA, MLA_ROPE,
               DIFF_HEADS * 2 * DIFF_HD, DIFF_HEADS * 2 * DIFF_HD, DIFF_HEADS * DIFF_VD,
               D_MODEL, D_MODEL)
N_IN = sum(SPLIT_SIZES)

kernel_name = 'hybrid_mla_diffattn_moe_encoder'


def _layernorm(x, g, b):
    xf = x.astype(jnp.float32)
    mu = jnp.mean(xf, axis=-1, keepdims=True)
    var = jnp.mean(jnp.square(xf - mu), axis=-1, keepdims=True)
    return ((xf - mu) * lax.rsqrt(var + LN_EPS) * g.astype(jnp.float32) + b.astype(jnp.float32)).astype(x.dtype)


def _rmsnorm(x, g):
    xf = x.astype(jnp.float32)
    ms = jnp.mean(jnp.square(xf), axis=-1, keepdims=True)
    return (xf * lax.rsqrt(ms + RMS_EPS) * g.astype(jnp.float32)).astype(x.dtype)


def _split_cols(z):
    out, off = [], 0
    for n in SPLIT_SIZES:
        out.append(z[..., off:off + n])
        off += n
    return out


def _rope_tables(positions):
    inv_freq = ROPE_BASE ** (-jnp.arange(0, MLA_ROPE, 2, dtype=jnp.float32) / MLA_ROPE)
    ang = positions.astype(jnp.float32)[..., None] * inv_freq
    return jnp.cos(ang), jnp.sin(ang)


def _rope(x, cos, sin):
    half = x.shape[-1] // 2
    x1 = x[..., :half].astype(jnp.float32)
    x2 = x[..., half:].astype(jnp.float32)
    return jnp.concatenate([x1 * cos - x2 * sin, x1 * sin + x2 * cos], axis=-1).astype(x.dtype)


def _alibi_slopes():
    return 2.0 ** (-8.0 * jnp.arange(1, DIFF_HEADS + 1, dtype=jnp.float32) / DIFF_HEADS)


def _query_blocks(a):
    b, s = a.shape[0], a.shape[1]
    a = a.reshape(b, s // Q_BLOCK, Q_BLOCK, *a.shape[2:])
    return jnp.moveaxis(a, 1, 0)


def _merge_blocks(a):
    a = jnp.moveaxis(a, 0, 1)
    return a.reshape(a.shape[0], a.shape[1] * a.shape[2], *a.shape[3:])


def _mla(q_lat, kv_lat, k_rope_raw, q_norm_g, w_q_up, kv_norm_g, w_kv_up, cos, sin):
    b, s = q_lat.shape[0], q_lat.shape[1]
    q = (_rmsnorm(q_lat, q_norm_g) @ w_q_up).reshape(b, s, MLA_HEADS, MLA_NOPE + MLA_ROPE)
    q_nope = q[..., :MLA_NOPE]
    q_rope = _rope(q[..., MLA_NOPE:], cos[:, :, None, :], sin[:, :, None, :])
    kv = (_rmsnorm(kv_lat, kv_norm_g) @ w_kv_up).reshape(b, s, MLA_HEADS, MLA_NOPE + MLA_V)
    k_nope = kv[..., :MLA_NOPE]
    v = kv[..., MLA_NOPE:]
    k_rope = _rope(k_rope_raw, cos, sin)
    scale = (MLA_NOPE + MLA_ROPE) ** -0.5

    def block(args):
        qn, qr = args
        sc = jnp.einsum('bqhd,bkhd->bhqk', qn, k_nope) + jnp.einsum('bqhr,bkr->bhqk', qr, k_rope)
        p = jax.nn.softmax(sc.astype(jnp.float32) * scale, axis=-1)
        return jnp.einsum('bhqk,bkhd->bqhd', p.astype(v.dtype), v)

    o = _merge_blocks(lax.map(block, (_query_blocks(q_nope), _query_blocks(q_rope))))
    return o.reshape(b, s, MLA_HEADS * MLA_V)


def _diff_attention(q, k, v, posf, lam, lam_init, norm_g, slopes):
    b, s = q.shape[0], q.shape[1]
    scale = DIFF_HD ** -0.5

    def block(args):
        qb, pq = args
        sc = jnp.einsum('bqhmd,bkhmd->mbhqk', qb, k).astype(jnp.float32) * scale
        dist = jnp.abs(pq[:, :, None] - posf[:, None, :])
        sc = sc - slopes[None, None, :, None, None] * dist[None, :, None, :, :]
        p = jax.nn.softmax(sc, axis=-1)
        a = p[0] - lam * p[1]
        return jnp.einsum('bhqk,bkhd->bqhd', a.astype(v.dtype), v)

    o = _merge_blocks(lax.map(block, (_query_blocks(q), _query_blocks(posf))))
    o = _rmsnorm(o, norm_g.reshape(DIFF_HEADS, DIFF_VD)) * (1.0 - lam_init)
    return o.reshape(b, s, DIFF_HEADS * DIFF_VD)


def _swiglu(h, w1, w3, w2):
    return (jax.nn.silu(h @ w1) * (h @ w3)) @ w2


def _moe(h, router_w, router_b, w1, w3, w2):
    b, s, d = h.shape
    t = b * s
    hf = h.reshape(t, d)
    logits = (hf @ router_w).astype(jnp.float32) + router_b.astype(jnp.float32)
    top_logits, top_idx = lax.top_k(logits, TOP_K)
    top_w = jax.nn.softmax(top_logits, axis=-1)
    expert_ids = top_idx.reshape(-1)
    token_ids = jnp.arange(t * TOP_K, dtype=jnp.int32) // TOP_K
    gates = top_w.reshape(-1)
    order = jnp.argsort(expert_ids)
    sorted_e = expert_ids[order]
    sorted_tok = token_ids[order]
    sorted_g = gates[order]
    counts = jnp.zeros((N_EXPERTS,), jnp.int32).at[expert_ids].add(1)
    starts = jnp.cumsum(counts) - counts
    padded = (counts + EXPERT_BLOCK - 1) // EXPERT_BLOCK * EXPERT_BLOCK
    pstarts = jnp.cumsum(padded) - padded
    pends = pstarts + padded
    dest = pstarts[sorted_e] + (jnp.arange(t * TOP_K, dtype=jnp.int32) - starts[sorted_e])
    n_rows = t * TOP_K + N_EXPERTS * EXPERT_BLOCK
    n_blk = n_rows // EXPERT_BLOCK
    buf_tok = jnp.full((n_rows,), t, jnp.int32).at[dest].set(sorted_tok)
    buf_g = jnp.zeros((n_rows,), jnp.float32).at[dest].set(sorted_g)
    blk_e = jnp.minimum(jnp.searchsorted(pends, jnp.arange(n_blk, dtype=jnp.int32) * EXPERT_BLOCK, side='right'),
                        N_EXPERTS - 1).astype(jnp.int32)
    xpad = jnp.concatenate([hf, jnp.zeros((1, d), hf.dtype)], axis=0)
    xb = xpad[buf_tok].reshape(n_blk, EXPERT_BLOCK, d)

    def expert_block(args):
        xe, e = args
        return (jax.nn.silu(xe @ w1[e]) * (xe @ w3[e])) @ w2[e]

    y = lax.map(expert_block, (xb, blk_e)).reshape(n_rows, d)
    y = y * buf_g[:, None].astype(y.dtype)
    out = jnp.zeros((t + 1, d), y.dtype).at[buf_tok].add(y)[:t]
    return out.reshape(b, s, d)


def setup_inputs(seed: int = 0) -> dict:
    key = jax.random.key(seed)
    keys = list(jax.random.split(key, 40))
    L, D, F, E = DEPTH, D_MODEL, D_FF, N_EXPERTS

    def nrm(shape, scale):
        return jax.random.normal(keys.pop(), shape, jnp.float32) * scale

    def gain(shape):
        return 1.0 + nrm(shape, 0.02)

    x = nrm((BATCH, SEQ, D), 1.0)
    c = nrm((BATCH, D), 1.0)
    offsets = jax.random.randint(keys.pop(), (BATCH, 1), 0, 4096, dtype=jnp.int32)
    positions = offsets + jnp.arange(SEQ, dtype=jnp.int32)[None, :]
    mla_out = MLA_HEADS * MLA_V
    diff_out = DIFF_HEADS * DIFF_VD
    return {
        'x': x,
        'c': c,
        'positions': positions,
        'w_ada': nrm((L, D, 6 * D), D ** -0.5),
        'b_ada': nrm((L, 6 * D), 0.02),
        'w_in': nrm((L, D, N_IN), D ** -0.5),
        'q_norm_g': gain((L, MLA_Q_LORA)),
        'w_q_up': nrm((L, MLA_Q_LORA, MLA_HEADS * (MLA_NOPE + MLA_ROPE)), MLA_Q_LORA ** -0.5),
        'kv_norm_g': gain((L, MLA_KV_LORA)),
        'w_kv_up': nrm((L, MLA_KV_LORA, MLA_HEADS * (MLA_NOPE + MLA_V)), MLA_KV_LORA ** -0.5),
        'lambda_q1': nrm((L, DIFF_HD), 0.1),
        'lambda_k1': nrm((L, DIFF_HD), 0.1),
        'lambda_q2': nrm((L, DIFF_HD), 0.1),
        'lambda_k2': nrm((L, DIFF_HD), 0.1),
        'diff_norm_g': gain((L, diff_out)),
        'w_br_mla': nrm((L, mla_out, D), mla_out ** -0.5 * DEEPNORM_BETA),
        'w_br_diff': nrm((L, diff_out, D), diff_out ** -0.5 * DEEPNORM_BETA),
        'w_out': nrm((L, D, D), D ** -0.5 * DEEPNORM_BETA),
        'ln1_g': gain((L, D)),
        'ln1_b': nrm((L, D), 0.02),
        'ln2_g': gain((L, D)),
        'ln2_b': nrm((L, D), 0.02),
        'ffn_w1': nrm((N_DENSE, D, F), D ** -0.5),
        'ffn_w3': nrm((N_DENSE, D, F), D ** -0.5),
        'ffn_w2': nrm((N_DENSE, F, D), F ** -0.5 * DEEPNORM_BETA),
        'router_w': nrm((N_MOE, D, E), D ** -0.5),
        'router_b': nrm((N_MOE, E), 0.01),
        'moe_w1': nrm((N_MOE, E, D, F), D ** -0.5),
        'moe_w3': nrm((N_MOE, E, D, F), D ** -0.5),
        'moe_w2': nrm((N_MOE, E, F, D), F ** -0.5 * DEEPNORM_BETA),
    }


def reference(x, c, positions, w_ada, b_ada, w_in, q_norm_g, w_q_up, kv_norm_g, w_kv_up,
              lambda_q1, lambda_k1, lambda_q2, lambda_k2, diff_norm_g, w_br_mla, w_br_diff, w_out,
              ln1_g, ln1_b, ln2_g, ln2_b, ffn_w1, ffn_w3, ffn_w2, router_w, router_b,
              moe_w1, moe_w3, moe_w2):
    b, s, _ = x.shape
    cos, sin = _rope_tables(positions)
    posf = positions.astype(jnp.float32)
    slopes = _alibi_slopes()
    cond = jax.nn.silu(c)
    for l in range(DEPTH):
        mod = cond @ w_ada[l] + b_ada[l]
        sh1, sc1, g1, sh2, sc2, g2 = jnp.split(mod[:, None, :], 6, axis=-1)
        h = x * (1.0 + sc1) + sh1
        z = h @ w_in[l]
        q_lat, kv_lat, k_rope, dq, dk, dv, gta, gtb = _split_cols(z)
        ya = _mla(q_lat, kv_lat, k_rope, q_norm_g[l], w_q_up[l], kv_norm_g[l], w_kv_up[l], cos, sin) @ w_br_mla[l]
        lam_init = 0.8 - 0.6 * math.exp(-0.3 * l)
        lam = (jnp.exp(jnp.sum(lambda_q1[l].astype(jnp.float32) * lambda_k1[l].astype(jnp.float32)))
               - jnp.exp(jnp.sum(lambda_q2[l].astype(jnp.float32) * lambda_k2[l].astype(jnp.float32)))
               + lam_init)
        yb = _diff_attention(dq.reshape(b, s, DIFF_HEADS, 2, DIFF_HD), dk.reshape(b, s, DIFF_HEADS, 2, DIFF_HD),
                             dv.reshape(b, s, DIFF_HEADS, DIFF_VD), posf, lam, lam_init, diff_norm_g[l], slopes) @ w_br_diff[l]
        mix = (jax.nn.sigmoid(gta) * ya + jax.nn.sigmoid(gtb) * yb) @ w_out[l]
        x = _layernorm(DEEPNORM_ALPHA * x + g1 * mix, ln1_g[l], ln1_b[l])
        h = x * (1.0 + sc2) + sh2
        if l % 2 == 0:
            f = _swiglu(h, ffn_w1[l // 2], ffn_w3[l // 2], ffn_w2[l // 2])
        else:
            f = _moe(h, router_w[l // 2], router_b[l // 2], moe_w1[l // 2], moe_w3[l // 2], moe_w2[l // 2])
        x = _layernorm(DEEPNORM_ALPHA * x + g2 * f, ln2_g[l], ln2_b[l])
    return x
```

```python
import math
import numpy as np
from contextlib import ExitStack
import concourse.bass as bass
import concourse.mybir as mybir
from concourse.bass_utils import run_bass_kernel_spmd

F32 = mybir.dt.float32
BF16 = mybir.dt.bfloat16
I32 = mybir.dt.int32
AF = mybir.ActivationFunctionType
ALU = mybir.AluOpType
AX = mybir.AxisListType

L = 4
D = 1024
S = 2048
NS = 2
F = 2816
NFC = 22
NEXP = 8
NE = 2 + 2 * NEXP
ALPHA = (2.0 * L) ** 0.25
LN_EPS = 1e-5
RMS_EPS = 1e-6
MLA_SCALE = 96.0 ** -0.5
DIFF_SCALE = 64.0 ** -0.5
TWO_PI = 2.0 * math.pi


class T:
    __slots__ = ("ap", "name", "last_w", "readers")

    def __init__(self, ap, name="", readers=None):
        self.ap = ap
        self.name = name
        self.last_w = None
        self.readers = dict(readers) if readers else {}

    def __getitem__(self, idx):
        return self.ap[idx]


def inherit(tiles):
    r = {}
    for t in tiles:
        for k, v in t.readers.items():
            r[k] = max(r.get(k, 0), v)
        if t.last_w is not None:
            k, v = t.last_w
            r[k] = max(r.get(k, 0), v)
    return r


class Em:
    NRING = 16
    SEM_LIMIT = 30000

    def __init__(self, nc):
        self.nc = nc
        self.eng = {"pe": nc.tensor, "act": nc.scalar, "dve": nc.vector,
                    "pool": nc.gpsimd, "sp": nc.sync}
        self.sem = {}
        self.cnt = {}
        self.known = {e: {} for e in self.eng}
        self.cur = {}
        self.nep = {}
        for e in self.eng:
            self.sem[e] = nc.alloc_semaphore(name="s_" + e)
            self.cnt[e] = 0
            self.cur[e] = e
            self.nep[e] = 0
        self.ring = {}
        for q in ("sp", "pool"):
            lst = []
            for i in range(self.NRING):
                key = "r_%s%d" % (q, i)
                self.sem[key] = nc.alloc_semaphore(name=key)
                self.cnt[key] = 0
                lst.append(key)
            self.ring[q] = [lst, 0]
        self.n_ins = {e: 0 for e in self.eng}
        self.n_wait = {e: 0 for e in self.eng}

    def _need(self, e, ev):
        if ev is None:
            return
        k, v = ev
        if e == "pe" and k.split("#")[0] == "pe":
            return
        if self.known[e].get(k, 0) >= v:
            return
        self.eng[e].wait_ge(self.sem[k], v)
        self.n_wait[e] += 1
        self.known[e][k] = v

    def _deps(self, e, reads, writes):
        for t in reads:
            self._need(e, t.last_w)
        for t in writes:
            self._need(e, t.last_w)
            for k, v in list(t.readers.items()):
                self._need(e, (k, v))

    def _commit(self, ev, reads, writes):
        k, v = ev
        for t in reads:
            if t.readers.get(k, 0) < v:
                t.readers[k] = v
        for t in writes:
            t.last_w = ev
            t.readers = {}

    def op(self, e, fn, reads=(), writes=()):
        self._deps(e, reads, writes)
        ins = fn()
        if e == "pe" and len(reads) > 0 and reads[0].last_w is not None and reads[0].last_w[0].split("#")[0] != "pe":
            k0, v0 = reads[0].last_w
            ins._wait_ge(self.sem[k0], v0)
        key = self.cur[e]
        if self.cnt[key] >= self.SEM_LIMIT:
            self.nep[e] += 1
            key = "%s#%d" % (e, self.nep[e])
            self.sem[key] = self.nc.alloc_semaphore(name="s_%s_%d" % (e, self.nep[e]))
            self.cnt[key] = 0
            self.cur[e] = key
        self.cnt[key] += 1
        ins.then_inc(self.sem[key], 1)
        ev = (key, self.cnt[key])
        self._commit(ev, reads, writes)
        self.n_ins[e] += 1
        return ev

    def dma(self, q, out_ap, in_ap, reads=(), writes=(), **kw):
        self._deps(q, reads, writes)
        lst, pos = self.ring[q]
        key = lst[pos % self.NRING]
        self.ring[q][1] = pos + 1
        if self.cnt[key] > 0:
            self._need(q, (key, self.cnt[key]))
        ins = self.eng[q].dma_start(out=out_ap, in_=in_ap, **kw)
        self.cnt[key] += 16
        ins.then_inc(self.sem[key], 16)
        ev = (key, self.cnt[key])
        self._commit(ev, reads, writes)
        self.n_ins[q] += 1
        return ev

    def finish(self, tiles):
        for t in tiles:
            self._need("sp", t.last_w)
        for e in self.eng:
            key = self.cur[e]
            if e != "sp" and self.cnt[key] > 0:
                self._need("sp", (key, self.cnt[key]))


class Rot:
    def __init__(self, items):
        self.items = items
        self.i = 0

    def next(self):
        t = self.items[self.i % len(self.items)]
        self.i += 1
        return t


def vlay():
    lay = {}
    off = 0
    for name, w in (("b_ada", L * 48), ("ln1_g", L * 8), ("ln1_b", L * 8), ("ln2_g", L * 8),
                    ("ln2_b", L * 8), ("qg", L * 3), ("kvg", L * 2), ("dng", L * 8),
                    ("rb", 2 * 8), ("freq", 1), ("nsgn", 1)):
        lay[name] = off
        off += w
    return lay, off


VL, NV = vlay()


def eidx(l, e):
    return (l // 2) if l % 2 == 0 else 2 + (l // 2) * NEXP + e


class Stop(Exception):
    pass


def build(n_layers=L, n_seq=NS, stop=None):
    nc = bass.Bass("TRN2", target_bir_lowering=False)
    es = ExitStack()

    def din(name, shape, dt=F32):
        return nc.dram_tensor(name, list(shape), dt, kind="ExternalInput").ap()

    Ld = n_layers
    NEd = max(eidx(l, e) for l in range(n_layers) for e in range(NEXP if l % 2 else 1)) + 1
    xT_d = din("xT", [NS, D, S])
    posb_d = din("posb", [NS, 128, S], I32)
    posk_d = din("posk", [NS, 128, 16], I32)
    cT_d = din("cT", [128, 8 * NS])
    vecs_d = din("vecs", [128, NV])
    sel_d = din("sel", [8, 8 * 128])
    ident_d = din("ident", [128, 128])
    lam_d = din("lam", [128, L * 256])
    w_ada_d = din("w_ada", [Ld, 48, 128, 1024])
    w_lat_d = din("w_lat", [Ld, 5, 128, 1024])
    w_kr_d = din("w_kr", [Ld, 2, 128, 8 * 96])
    w_dq_d = din("w_dq", [Ld, 8, 128, 1024])
    w_dk_d = din("w_dk", [Ld, 8, 128, 1024])
    w_dv_d = din("w_dv", [Ld, 8, 128, 1024])
    w_gt_d = din("w_gt", [Ld, 16, 128, 1024])
    w_q_d = din("w_q", [Ld, 8, 2, 128, 3 * 96])
    w_kn_d = din("w_kn", [Ld, 8, 128, 128])
    w_v_d = din("w_v", [Ld, 8, 128, 128])
    w_brm_d = din("w_brm", [Ld, 8, 128, 512])
    w_brd_d = din("w_brd", [Ld, 8, 128, 1024])
    w_out_d = din("w_out", [Ld, 8, 128, 1024])
    w1_d = din("w1", [NEd, NFC, 128, 1024])
    w3_d = din("w3", [NEd, NFC, 128, 1024])
    w2_d = din("w2", [NEd, 8, 3, 128, 1024])
    rw_d = din("rw", [2, 128, 64])
    outT_d = nc.dram_tensor("outT", [NS, D, S], F32, kind="ExternalOutput").ap()
    etab_d = nc.dram_tensor("etab", [NS, 8, S, S], BF16).ap()
    otm_d = nc.dram_tensor("otm", [512, S], BF16).ap()
    otd_d = nc.dram_tensor("otd", [1024, S], BF16).ap()

    em = Em(nc)
    E = em

    def sbt(name, shape, dt):
        return es.enter_context(nc.sbuf_tensor("sb_" + name, list(shape), dt))

    def sb(name, shape, dt):
        return T(sbt(name, shape, dt), name)

    def pst(name, shape, dt):
        return es.enter_context(nc.psum_tensor("ps_" + name, list(shape), dt))

    Din = T(None, "dram_in")
    Tout = [T(None, "out%d" % s) for s in range(NS)]
    Tetab = [[T(None, "etab") for h in range(8)] for s in range(NS)]
    Totm = [T(None, "otm%d" % tb) for tb in range(4)]
    Totd = [T(None, "otd%d" % tb) for tb in range(4)]

    xT_t = sbt("xT_t", [128, 8 * S], F32)
    hT_t = sbt("hT_t", [128, 8 * S], BF16)
    xT = [[T(xT_t[:, c * S + tb * 512: c * S + (tb + 1) * 512], "x%d_%d" % (c, tb)) for tb in range(4)]
          for c in range(8)]
    hT = [[T(hT_t[:, c * S + tb * 512: c * S + (tb + 1) * 512], "h%d_%d" % (c, tb)) for tb in range(4)]
          for c in range(8)]
    vecs = sb("vecs", [128, NV], F32)
    cT = sb("cT", [128, 8 * NS], F32)
    cond = sb("cond", [128, 8 * NS], F32)
    mod = sb("mod", [128, L * 48 * NS], F32)
    NDV = 8
    dvt = sb("dvt", [128, L * NS * NDV * 8], F32)
    nlam = sb("nlam", [128, L], F32)
    lamtmp = sb("lamtmp", [128, 64 + 4], F32)
    ones_bf = sb("ones_bf", [128, 128], BF16)
    ones_f = sb("ones_f", [128, 128], F32)
    ident_bf = sb("ident_bf", [128, 128], BF16)
    ident_f = sb("ident_f", [128, 128], F32)
    sel = sb("sel", [8, 8 * 128], BF16)
    rw = [sb("rw%d" % i, [128, 64], F32) for i in range(2)]
    rwp = sb("rwp", [128, 64], F32)
    brow = sb("brow", [1, 8], F32)
    tabC = sb("tabC", [128, S], BF16)
    tabS = sb("tabS", [128, S], BF16)
    posk_i = sb("posk_i", [128, 16], I32)
    posk = sb("posk", [128, 16], F32)
    NWS = 8
    wslots = Rot([sb("wsl%d" % i, [128, 1024], BF16) for i in range(NWS)])
    wstage = Rot([sb("wstg%d" % i, [128, 1024], F32) for i in range(2)])
    PTs = Rot([sb("PT%d" % i, [128, 512], BF16) for i in range(3)])
    Ets = Rot([sb("Et%d" % i, [128, 512], BF16) for i in range(2)])
    OTst = Rot([sb("OTst%d" % i, [128, 512], BF16) for i in range(2)])
    tmpA = Rot([sb("tmpA%d" % i, [128, 512], F32) for i in range(3)])
    tmpB = Rot([sb("tmpB%d" % i, [128, 512], BF16) for i in range(3)])
    small = Rot([sb("small%d" % i, [128, 16], F32) for i in range(6)])
    lgt = Rot([sb("lgt%d" % i, [128, 64], F32) for i in range(2)])
    GT = sb("GT", [8, 1024], BF16)
    gb = Rot([sb("gb%d" % i, [128, 1024], BF16) for i in range(1)])
    ARENA_N = 22560
    arena = sbt("arena", [128, ARENA_N], BF16)

    pb = [T(pst("pb%d" % i, [128, 512], F32), "pb%d" % i) for i in range(7)]
    ptb = T(pst("ptb", [128, 1024], BF16), "ptb")
    gp = Rot(pb[0:4])
    gp7 = Rot(pb)

    def mm(out_ap, lhsT, rhs, start, stop, reads, writes, **kw):
        E.op("pe", lambda: nc.tensor.matmul(out_ap, lhsT, rhs, start=start, stop=stop, **kw), reads, writes)

    def act(out_ap, in_ap, func, reads, writes, **kw):
        E.op("act", lambda: nc.scalar.activation(out_ap, in_ap, func, **kw), reads, writes)

    def dve(fn, reads, writes):
        E.op("dve", fn, reads, writes)

    def V(name):
        return VL[name]

    def vcol(name, i):
        o = VL[name] + i
        return vecs[:, o:o + 1]

    wcnt = [0]

    def wload(dram_ap, n, reads=(Din,)):
        st_ = wstage.next()
        E.dma("sp", st_[:, 0:n], dram_ap, reads=list(reads), writes=[st_])
        w = wslots.next()
        wcnt[0] += 1
        if wcnt[0] % 2 == 0:
            dve(lambda: nc.vector.tensor_copy(w[:, 0:n], st_[:, 0:n]), [st_], [w])
        else:
            act(w[:, 0:n], st_[:, 0:n], AF.Copy, [st_], [w])
        return w

    def dvcol(l, s, k, c):
        o = ((l * NS + s) * NDV + k) * 8 + c
        return dvt[:, o:o + 1]

    def dvvec(l, s, k):
        o = ((l * NS + s) * NDV + k) * 8
        return dvt[:, o:o + 8]

    def modvec(l, blk, s):
        o = (l * 6 + blk) * 8 * NS
        return mod[:, o:o + 8 * NS].rearrange("p (m s) -> p m s", s=NS)[:, :, s]

    def modcol(l, blk, s, c):
        o = ((l * 6 + blk) * 8 + c) * NS + s
        return mod[:, o:o + 1]

    A1, G1, A2, G2, GA2, GB2, GAN, GBN = range(8)

    E.dma("sp", vecs[:, :], vecs_d[:, :], reads=[Din], writes=[vecs])
    E.dma("sp", cT[:, :], cT_d[:, :], reads=[Din], writes=[cT])
    self_ = wstage.next()
    E.dma("sp", self_[0:8, :], sel_d[:, :], reads=[Din], writes=[self_])
    dve(lambda: nc.vector.tensor_copy(sel[:, :], self_[0:8, :]), [self_], [sel])
    E.dma("sp", ident_f[:, :], ident_d[:, :], reads=[Din], writes=[ident_f])
    for i in range(2):
        E.dma("sp", rw[i][:, :], rw_d[i], reads=[Din], writes=[rw[i]])
    dve(lambda: nc.vector.memset(ones_bf[:, :], 1.0), [], [ones_bf])
    dve(lambda: nc.vector.memset(ones_f[:, :], 1.0), [], [ones_f])
    dve(lambda: nc.vector.tensor_copy(ident_bf[:, :], ident_f[:, :]), [ident_f], [ident_bf])
    act(cond[:, :], cT[:, :], AF.Silu, [cT], [cond])
    cond_bf = sb("cond_bf", [128, 8 * NS], BF16)
    dve(lambda: nc.vector.tensor_copy(cond_bf[:, :], cond[:, :]), [cond], [cond_bf])
    rwb = [sb("rwb%d" % i, [128, 64], BF16) for i in range(2)]
    for i in range(2):
        dve(lambda: nc.vector.tensor_copy(rwb[i][:, :], rw[i][:, :]), [rw[i]], [rwb[i]])
    lgb = sb("lgb", [128, 8], BF16)

    for l in range(n_layers):
        for blk in range(6):
            for m in range(8):
                w = wload(w_ada_d[l, blk * 8 + m], 1024)
                p = gp.next()
                for kc in range(8):
                    mm(p[:, 0:NS], w[:, kc * 128:(kc + 1) * 128], cond_bf[:, kc * NS:(kc + 1) * NS],
                       kc == 0, kc == 7, [w, cond_bf], [p])
                o = ((l * 6 + blk) * 8 + m) * NS
                dve(lambda: nc.vector.tensor_scalar(mod[:, o:o + NS], p[:, 0:NS], vcol("b_ada", l * 48 + blk * 8 + m),
                                                    None, ALU.add), [p, vecs], [mod])
    for l in range(n_layers):
        for s in range(n_seq):
            dve(lambda: nc.vector.tensor_scalar(dvvec(l, s, A1), modvec(l, 1, s), 1.0, None, ALU.add), [mod], [dvt])
            dve(lambda: nc.vector.tensor_scalar(dvvec(l, s, G1), modvec(l, 2, s), 1.0 / ALPHA, None, ALU.mult), [mod], [dvt])
            dve(lambda: nc.vector.tensor_scalar(dvvec(l, s, A2), modvec(l, 4, s), 1.0, None, ALU.add), [mod], [dvt])
            dve(lambda: nc.vector.tensor_scalar(dvvec(l, s, G2), modvec(l, 5, s), 1.0 / ALPHA, None, ALU.mult), [mod], [dvt])
            g1v = vecs[:, V("ln1_g") + l * 8: V("ln1_g") + l * 8 + 8]
            b1v = vecs[:, V("ln1_b") + l * 8: V("ln1_b") + l * 8 + 8]
            dve(lambda: nc.vector.tensor_tensor(dvvec(l, s, GA2), g1v, dvvec(l, s, A2), ALU.mult), [vecs, dvt], [dvt])
            dve(lambda: nc.vector.tensor_tensor(dvvec(l, s, GB2), b1v, dvvec(l, s, A2), ALU.mult), [vecs, dvt], [dvt])
            dve(lambda: nc.vector.tensor_tensor(dvvec(l, s, GB2), dvvec(l, s, GB2), modvec(l, 3, s), ALU.add), [mod, dvt], [dvt])
    for l in range(n_layers - 1):
        for s in range(n_seq):
            g2v = vecs[:, V("ln2_g") + l * 8: V("ln2_g") + l * 8 + 8]
            b2v = vecs[:, V("ln2_b") + l * 8: V("ln2_b") + l * 8 + 8]
            dve(lambda: nc.vector.tensor_tensor(dvvec(l, s, GAN), g2v, dvvec(l + 1, s, A1), ALU.mult), [vecs, dvt], [dvt])
            dve(lambda: nc.vector.tensor_tensor(dvvec(l, s, GBN), b2v, dvvec(l + 1, s, A1), ALU.mult), [vecs, dvt], [dvt])
            dve(lambda: nc.vector.tensor_tensor(dvvec(l, s, GBN), dvvec(l, s, GBN), modvec(l + 1, 0, s), ALU.add), [mod, dvt], [dvt])
    lamv = T(arena[:, 8192:8192 + 2 * L * 256].bitcast(F32), "lamv")
    E.dma("sp", lamv[:, :], lam_d[:, :], reads=[Din], writes=[lamv])
    for l in range(n_layers):
        lam_init = 0.8 - 0.6 * math.exp(-0.3 * l)
        lo = l * 256
        for j in range(2):
            dve(lambda: nc.vector.tensor_tensor(lamtmp[:, 0:64], lamv[:, lo + j * 128: lo + j * 128 + 64],
                                                lamv[:, lo + j * 128 + 64: lo + j * 128 + 128], ALU.mult), [lamv], [lamtmp])
            dve(lambda: nc.vector.reduce_sum(lamtmp[:, 64 + j:65 + j], lamtmp[:, 0:64], axis=AX.X), [lamtmp], [lamtmp])
            act(lamtmp[:, 66 + j:67 + j], lamtmp[:, 64 + j:65 + j], AF.Exp, [lamtmp], [lamtmp])
        dve(lambda: nc.vector.tensor_tensor(lamtmp[:, 64:65], lamtmp[:, 67:68], lamtmp[:, 66:67], ALU.subtract), [lamtmp], [lamtmp])
        dve(lambda: nc.vector.tensor_scalar(nlam[:, l:l + 1], lamtmp[:, 64:65], -lam_init, None, ALU.add), [lamtmp], [nlam])

    arena_prev = [lamv]
    hprev = []

    def layernorm(tb, lng, lnb, l, GAk, GBk, s, write_h):
        psum_s = pb[4]
        psum_q = pb[5]
        for dc in range(8):
            ub = tmpB.next()
            act(ub[:, :], xT[dc][tb][:, :], AF.Copy, [xT[dc][tb]], [ub])
            mm(psum_s[:, :], ones_bf[:, :], ub[:, :], dc == 0, dc == 7, [ones_bf, ub], [psum_s])
            uq = tmpB.next()
            act(uq[:, :], xT[dc][tb][:, :], AF.Square, [xT[dc][tb]], [uq])
            mm(psum_q[:, :], ones_bf[:, :], uq[:, :], dc == 0, dc == 7, [ones_bf, uq], [psum_q])
        mean = tmpA.next()
        dve(lambda: nc.vector.tensor_scalar(mean[:, :], psum_s[:, :], 1.0 / D, None, ALU.mult), [psum_s], [mean])
        msq = tmpA.next()
        dve(lambda: nc.vector.tensor_tensor(msq[:, :], mean[:, :], mean[:, :], ALU.mult), [mean], [msq])
        dve(lambda: nc.vector.scalar_tensor_tensor(msq[:, :], psum_q[:, :], 1.0 / D, msq[:, :], ALU.mult, ALU.subtract),
            [psum_q, msq], [msq])
        act(msq[:, :], msq[:, :], AF.Sqrt, [msq], [msq], bias=LN_EPS / (ALPHA * ALPHA), scale=1.0)
        rstd = tmpA.next()
        dve(lambda: nc.vector.reciprocal(rstd[:, :], msq[:, :]), [msq], [rstd])
        for dc in range(8):
            x_ = xT[dc][tb]
            dve(lambda: nc.vector.tensor_tensor(x_[:, :], x_[:, :], mean[:, :], ALU.subtract), [x_, mean], [x_])
            dve(lambda: nc.vector.tensor_tensor(x_[:, :], x_[:, :], rstd[:, :], ALU.mult), [x_, rstd], [x_])
            if write_h:
                h_ = hT[dc][tb]
                dve(lambda: nc.vector.tensor_scalar(h_[:, :], x_[:, :], dvcol(l, s, GAk, dc), dvcol(l, s, GBk, dc),
                                                    ALU.mult, ALU.add), [x_, dvt], [h_])
            dve(lambda: nc.vector.tensor_scalar(x_[:, :], x_[:, :], vcol(lng, l * 8 + dc), vcol(lnb, l * 8 + dc),
                                                ALU.mult, ALU.add), [x_, vecs], [x_])

    gps = Rot(pb[0:3])

    def attention(QT, KT, Vt, krows, scale, nmaps, dv, s, h, finish_fn):
        Vv = Vt[:, 0:16 * dv].rearrange("p (t d) -> p t d", d=dv)
        accO = [pb[3], pb[5]]
        accS = [pb[4], pb[6]]
        for qb in range(4):
            qs = slice(qb * 512, (qb + 1) * 512)
            for kt in range(16):
                ks = slice(kt * 128, (kt + 1) * 128)
                PTl = []
                if nmaps == 2:
                    et = Ets.next()
                    E.dma("sp", et[:, :], etab_d[s, h, kt * 128:(kt + 1) * 128, qs], reads=[Tetab[s][h]], writes=[et])
                for m in range(nmaps):
                    p = gps.next()
                    if nmaps == 2:
                        rows = slice(m * 64, (m + 1) * 64)
                    else:
                        rows = slice(0, krows)
                    mm(p[:, :], KT[rows, ks], QT[rows, qs], True, True, [KT, QT], [p])
                    pt = PTs.next()
                    act(pt[:, :], p[:, :], AF.Exp, [p], [pt], scale=scale)
                    if nmaps == 2:
                        dve(lambda: nc.vector.tensor_tensor(pt[:, :], pt[:, :], et[:, :], ALU.mult), [pt, et], [pt])
                    PTl.append(pt)
                for m in range(nmaps):
                    mm(accO[m][0:dv, :], Vv[:, kt, :], PTl[m][:, :], kt == 0, kt == 15, [Vt, PTl[m]], [accO[m]])
                    mm(accS[m][0:dv, :], ones_bf[:, 0:dv], PTl[m][:, :], kt == 0, kt == 15, [ones_bf, PTl[m]], [accS[m]])
            finish_fn(qb, accO, accS)

    def run_seq(s):
        nonlocal arena_prev
        for c in range(8):
            for tb in range(4):
                E.dma("sp", xT[c][tb][:, :], xT_d[s, c * 128:(c + 1) * 128, tb * 512:(tb + 1) * 512],
                      reads=[Din], writes=[xT[c][tb]])
        rd = inherit(arena_prev)
        posb_i = T(arena[:, 0:4096].bitcast(I32), "posb_i", rd)
        posb = T(arena[:, 4096:8192].bitcast(F32), "posb", rd)
        ang = T(arena[:, 8192:12288].bitcast(F32), "ang", rd)
        ang2 = T(arena[:, 12288:16384].bitcast(F32), "ang2", rd)
        etmp = Rot([T(arena[:, 16384 + i * 2048:16384 + (i + 1) * 2048], "etmp%d" % i, rd) for i in range(2)])
        E.dma("sp", posb_i[:, :], posb_d[s], reads=[Din], writes=[posb_i])
        E.dma("sp", posk_i[:, :], posk_d[s], reads=[Din], writes=[posk_i])
        dve(lambda: nc.vector.tensor_copy(posb[:, :], posb_i[:, :]), [posb_i], [posb])
        dve(lambda: nc.vector.tensor_copy(posk[:, :], posk_i[:, :]), [posk_i], [posk])
        angi = T(arena[:, 20480:20480 + 2048].bitcast(I32), "angi", rd)

        def reduce_angle(dst, src, shift):
            for hf in range(2):
                cs = slice(hf * 1024, (hf + 1) * 1024)
                dve(lambda: nc.vector.tensor_scalar(dst[:, cs], src[:, cs], shift, 1.0 / TWO_PI, ALU.add, ALU.mult), [src], [dst])
                dve(lambda: nc.vector.tensor_copy(angi[:, :], dst[:, cs]), [dst], [angi])
                dve(lambda: nc.vector.tensor_copy(dst[:, cs], angi[:, :]), [angi], [dst])
                dve(lambda: nc.vector.scalar_tensor_tensor(dst[:, cs], dst[:, cs], -TWO_PI, src[:, cs], ALU.mult, ALU.add), [dst, src], [dst])
                dve(lambda: nc.vector.tensor_scalar(dst[:, cs], dst[:, cs], -3.1415925 - shift, 3.1415925 - shift, ALU.max, ALU.min), [dst], [dst])
                dve(lambda: nc.vector.tensor_scalar(dst[:, cs], dst[:, cs], shift, None, ALU.add), [dst], [dst])
        dve(lambda: nc.vector.tensor_scalar(ang[:, :], posb[:, :], vcol("freq", 0), None, ALU.mult), [posb, vecs], [ang])
        reduce_angle(ang2, ang, math.pi / 2)
        act(tabC[:, :], ang2[:, :], AF.Sin, [ang2], [tabC])
        reduce_angle(ang2, ang, 0.0)
        act(ang[:, :], ang2[:, :], AF.Sin, [ang2], [ang])
        dve(lambda: nc.vector.tensor_scalar(tabS[:, :], ang[:, :], vcol("nsgn", 0), None, ALU.mult), [ang, vecs], [tabS])
        dve(lambda: nc.vector.tensor_scalar(posk[:, :], posk[:, :], -1.0, None, ALU.mult), [posk], [posk])
        for kt in range(16):
            act(ang[:, :], posb[:, :], AF.Abs, [posb, posk], [ang], bias=posk[:, kt:kt + 1], scale=1.0)
            for h in range(8):
                slope = 2.0 ** (-(h + 1))
                et = etmp.next()
                act(et[:, :], ang[:, :], AF.Exp, [ang], [et], scale=-slope)
                E.dma("sp", etab_d[s, h, kt * 128:(kt + 1) * 128, :], et[:, :], reads=[et], writes=[Tetab[s][h]])
        arena_prev = [posb_i, posb, ang, ang2, angi] + etmp.items

        for l in range(n_layers):
            lam_init = 0.8 - 0.6 * math.exp(-0.3 * l)
            if stop == 'tables':
                raise Stop()
            if l == 0:
                for c in range(8):
                    for tb in range(4):
                        dve(lambda: nc.vector.tensor_scalar(hT[c][tb][:, :], xT[c][tb][:, :], dvcol(l, s, A1, c),
                                                            modcol(l, 0, s, c), ALU.mult, ALU.add),
                            [xT[c][tb], dvt, mod], [hT[c][tb]])

            if stop == 'p1':
                raise Stop()
            rd = inherit(arena_prev)
            latn = [[T(arena[:, (j * 4 + tb) * 512:(j * 4 + tb + 1) * 512], "latn", rd) for tb in range(4)] for j in range(5)]
            o0 = 5 * 2048
            krT = T(arena[:, o0:o0 + S], "krT", rd)
            o0 += S
            QTm = Rot([T(arena[:, o0 + i * S:o0 + (i + 1) * S], "QTm%d" % i, rd) for i in range(2)])
            o0 += 2 * S
            KTm = Rot([T(arena[:, o0 + i * S:o0 + (i + 1) * S], "KTm%d" % i, rd) for i in range(2)])
            o0 += 2 * S
            Vm = Rot([T(arena[:, o0 + i * 1040:o0 + (i + 1) * 1040], "Vm%d" % i, rd) for i in range(2)])
            o0 += 2 * 1040
            assert o0 <= ARENA_N

            wl = [wload(w_lat_d[l, j], 1024) for j in range(5)]
            for tb in range(4):
                for grp, (j0, j1, n, gname) in enumerate(((0, 3, 384, "qg"), (3, 5, 256, "kvg"))):
                    pq = pb[6]
                    for j in range(j0, j1):
                        p = gp.next()
                        for kc in range(8):
                            mm(p[:, :], wl[j][:, kc * 128:(kc + 1) * 128], hT[kc][tb][:, :], kc == 0, kc == 7,
                               [wl[j], hT[kc][tb]], [p])
                        sq = tmpB.next()
                        dve(lambda: nc.vector.tensor_copy(latn[j][tb][:, :], p[:, :]), [p], [latn[j][tb]])
                        act(sq[:, :], latn[j][tb][:, :], AF.Square, [latn[j][tb]], [sq])
                        mm(pq[:, :], ones_bf[:, :], sq[:, :], j == j0, j == j1 - 1, [ones_bf, sq], [pq])
                    sd = tmpA.next()
                    act(sd[:, :], pq[:, :], AF.Sqrt, [pq], [sd], bias=RMS_EPS, scale=1.0 / n)
                    dve(lambda: nc.vector.reciprocal(sd[:, :], sd[:, :]), [sd], [sd])
                    for j in range(j0, j1):
                        gi = (l * 3 + j) if grp == 0 else (l * 2 + j - 3)
                        dve(lambda: nc.vector.scalar_tensor_tensor(latn[j][tb][:, :], latn[j][tb][:, :], vcol(gname, gi),
                                                                   sd[:, :], ALU.mult, ALU.mult),
                            [latn[j][tb], vecs, sd], [latn[j][tb]])

            if stop == 'lat':
                raise Stop()
            wka = wload(w_kr_d[l, 0], 768)
            wkb = wload(w_kr_d[l, 1], 768)
            for tb in range(4):
                ts_ = slice(tb * 512, (tb + 1) * 512)
                pa = gp.next()
                pb_ = gp.next()
                for kc in range(8):
                    mm(pa[0:96, :], wka[:, kc * 96:(kc + 1) * 96], hT[kc][tb][:, :], kc == 0, kc == 7, [wka, hT[kc][tb]], [pa])
                for kc in range(8):
                    mm(pb_[0:96, :], wkb[:, kc * 96:(kc + 1) * 96], hT[kc][tb][:, :], kc == 0, kc == 7, [wkb, hT[kc][tb]], [pb_])
                t1 = tmpA.next()
                t2 = tmpA.next()
                dve(lambda: nc.vector.tensor_tensor(t1[0:96, :], pa[0:96, :], tabC[0:96, ts_], ALU.mult), [pa, tabC], [t1])
                dve(lambda: nc.vector.tensor_tensor(t2[0:96, :], pb_[0:96, :], tabS[0:96, ts_], ALU.mult), [pb_, tabS], [t2])
                dve(lambda: nc.vector.tensor_tensor(krT[0:96, ts_], t1[0:96, :], t2[0:96, :], ALU.add), [t1, t2], [krT])

            if stop == 'kr':
                raise Stop()
            for h in range(8):
                wqa = wload(w_q_d[l, h, 0], 288)
                wqb = wload(w_q_d[l, h, 1], 288)
                wkn = wload(w_kn_d[l, h], 128)
                wv = wload(w_v_d[l, h], 128)
                QT = QTm.next()
                KT = KTm.next()
                Vt = Vm.next()
                for tb in range(4):
                    ts_ = slice(tb * 512, (tb + 1) * 512)
                    pa = gp.next()
                    pb_ = gp.next()
                    for kc in range(3):
                        mm(pa[0:96, :], wqa[:, kc * 96:(kc + 1) * 96], latn[kc][tb][:, :], kc == 0, kc == 2, [wqa, latn[kc][tb]], [pa])
                    for kc in range(3):
                        mm(pb_[0:96, :], wqb[:, kc * 96:(kc + 1) * 96], latn[kc][tb][:, :], kc == 0, kc == 2, [wqb, latn[kc][tb]], [pb_])
                    t1 = tmpA.next()
                    t2 = tmpA.next()
                    dve(lambda: nc.vector.tensor_tensor(t1[0:96, :], pa[0:96, :], tabC[0:96, ts_], ALU.mult), [pa, tabC], [t1])
                    dve(lambda: nc.vector.tensor_tensor(t2[0:96, :], pb_[0:96, :], tabS[0:96, ts_], ALU.mult), [pb_, tabS], [t2])
                    dve(lambda: nc.vector.tensor_tensor(QT[0:96, ts_], t1[0:96, :], t2[0:96, :], ALU.add), [t1, t2], [QT])
                    pk = gp.next()
                    for kc in range(2):
                        mm(pk[0:64, :], wkn[:, kc * 64:(kc + 1) * 64], latn[3 + kc][tb][:, :], kc == 0, kc == 1,
                           [wkn, latn[3 + kc][tb]], [pk])
                    act(KT[0:64, ts_], pk[0:64, :], AF.Copy, [pk], [KT])
                    dve(lambda: nc.vector.tensor_copy(KT[64:96, ts_], krT[64:96, ts_]), [krT], [KT])
                Vv = Vt[:, 0:1024].rearrange("p (t d) -> p t d", d=64)
                for tg in range(4):
                    pv = gp.next()
                    for ti in range(4):
                        tt = tg * 4 + ti
                        tb, off = tt // 4, (tt % 4) * 128
                        for kc in range(2):
                            mm(pv[:, ti * 64:(ti + 1) * 64], latn[3 + kc][tb][:, off:off + 128], wv[:, kc * 64:(kc + 1) * 64],
                               (kc == 0 and ti == 0), kc == 1, [latn[3 + kc][tb], wv], [pv], skip_group_check=True)
                    dve(lambda: nc.vector.tensor_copy(Vv[:, tg * 4:(tg + 1) * 4, :],
                                                      pv[:, 0:256].rearrange("p (t d) -> p t d", d=64)), [pv], [Vt])

                def fin_mla(qb, accO, accS, h=h):
                    r = tmpA.next()
                    dve(lambda: nc.vector.reciprocal(r[0:64, :], accS[0][0:64, :]), [accS[0]], [r])
                    st = OTst.next()
                    dve(lambda: nc.vector.tensor_tensor(st[0:64, :], accO[0][0:64, :], r[0:64, :], ALU.mult), [accO[0], r], [st])
                    E.dma("sp", otm_d[h * 64:(h + 1) * 64, qb * 512:(qb + 1) * 512], st[0:64, :], reads=[st], writes=[Totm[qb]])

                attention(QT, KT, Vt, 96, MLA_SCALE, 1, 64, s, h, fin_mla)

            if stop == 'mla':
                raise Stop()
            rd = inherit([t for row in latn for t in row] + [krT] + QTm.items + KTm.items + Vm.items)
            o0 = 0
            QTd = Rot([T(arena[:, o0 + i * S:o0 + (i + 1) * S], "QTd%d" % i, rd) for i in range(2)])
            o0 += 2 * S
            KTd = Rot([T(arena[:, o0 + i * S:o0 + (i + 1) * S], "KTd%d" % i, rd) for i in range(2)])
            o0 += 2 * S
            Vd = Rot([T(arena[:, o0 + i * 2064:o0 + (i + 1) * 2064], "Vd%d" % i, rd) for i in range(2)])
            o0 += 2 * 2064
            assert o0 <= ARENA_N
            for h in range(8):
                wdq = wload(w_dq_d[l, h], 1024)
                wdk = wload(w_dk_d[l, h], 1024)
                wdv = wload(w_dv_d[l, h], 1024)
                QT = QTd.next()
                KT = KTd.next()
                Vt = Vd.next()
                for tb in range(4):
                    ts_ = slice(tb * 512, (tb + 1) * 512)
                    for (w_, dst, eng_) in ((wdq, QT, "act"), (wdk, KT, "dve")):
                        p = gp.next()
                        for kc in range(8):
                            mm(p[:, :], w_[:, kc * 128:(kc + 1) * 128], hT[kc][tb][:, :], kc == 0, kc == 7, [w_, hT[kc][tb]], [p])
                        if eng_ == "act":
                            act(dst[:, ts_], p[:, :], AF.Copy, [p], [dst])
                        else:
                            dve(lambda: nc.vector.tensor_copy(dst[:, ts_], p[:, :]), [p], [dst])
                Vv = Vt[:, 0:2048].rearrange("p (t d) -> p t d", d=128)
                for tg in range(4):
                    pv = gp.next()
                    for ti in range(4):
                        tt = tg * 4 + ti
                        tb, off = tt // 4, (tt % 4) * 128
                        for kc in range(8):
                            mm(pv[:, ti * 128:(ti + 1) * 128], hT[kc][tb][:, off:off + 128], wdv[:, kc * 128:(kc + 1) * 128],
                               (kc == 0 and ti == 0), kc == 7, [hT[kc][tb], wdv], [pv], skip_group_check=True)
                    act(Vv[:, tg * 4:(tg + 1) * 4, :], pv[:, :].rearrange("p (t d) -> p t d", d=128), AF.Copy, [pv], [Vt])

                def fin_diff(qb, accO, accS, h=h, l=l, lam_init=lam_init):
                    r0, r1, o0 = tmpA.next(), tmpA.next(), tmpA.next()
                    dve(lambda: nc.vector.reciprocal(r0[:, :], accS[0][:, :]), [accS[0]], [r0])
                    dve(lambda: nc.vector.reciprocal(r1[:, :], accS[1][:, :]), [accS[1]], [r1])
                    dve(lambda: nc.vector.tensor_tensor(o0[:, :], accO[0][:, :], r0[:, :], ALU.mult), [accO[0], r0], [o0])
                    dve(lambda: nc.vector.scalar_tensor_tensor(r1[:, :], accO[1][:, :], nlam[:, l:l + 1], r1[:, :], ALU.mult, ALU.mult),
                        [accO[1], nlam, r1], [r1])
                    dve(lambda: nc.vector.tensor_tensor(o0[:, :], o0[:, :], r1[:, :], ALU.add), [o0, r1], [o0])
                    sq = tmpB.next()
                    act(sq[:, :], o0[:, :], AF.Square, [o0], [sq])
                    pr = gps.next()
                    mm(pr[:, :], ones_bf[:, :], sq[:, :], True, True, [ones_bf, sq], [pr])
                    k2 = (1.0 - lam_init) ** 2
                    act(r0[:, :], pr[:, :], AF.Sqrt, [pr], [r0], bias=RMS_EPS / k2, scale=1.0 / (128.0 * k2))
                    dve(lambda: nc.vector.reciprocal(r0[:, :], r0[:, :]), [r0], [r0])
                    st = OTst.next()
                    dve(lambda: nc.vector.scalar_tensor_tensor(st[:, :], o0[:, :], vcol("dng", l * 8 + h), r0[:, :], ALU.mult, ALU.mult),
                        [o0, vecs, r0], [st])
                    E.dma("sp", otd_d[h * 128:(h + 1) * 128, qb * 512:(qb + 1) * 512], st[:, :], reads=[st], writes=[Totd[qb]])

                attention(QT, KT, Vt, 64, DIFF_SCALE, 2, 128, s, h, fin_diff)

            if stop == 'diff':
                raise Stop()
            rd = inherit(QTd.items + KTd.items + Vd.items)
            otmT = T(arena[:, 0:2048], "otmT", rd)
            otdT = T(arena[:, 2048:6144], "otdT", rd)
            mrg = [T(arena[:, 6144 + c * 512:6144 + (c + 1) * 512], "mrg%d" % c, rd) for c in range(8)]
            is_moe = (l % 2 == 1)
            for tb in range(4):
                ts_ = slice(tb * 512, (tb + 1) * 512)
                for kc in range(4):
                    E.dma("sp", otmT[:, kc * 512:(kc + 1) * 512], otm_d[kc * 128:(kc + 1) * 128, ts_], reads=[Totm[tb]], writes=[otmT])
                for kc in range(8):
                    E.dma("sp", otdT[:, kc * 512:(kc + 1) * 512], otd_d[kc * 128:(kc + 1) * 128, ts_], reads=[Totd[tb]], writes=[otdT])
                for dc in range(8):
                    wbm = wload(w_brm_d[l, dc], 512)
                    wbd = wload(w_brd_d[l, dc], 1024)
                    wga = wload(w_gt_d[l, dc], 1024)
                    wgb = wload(w_gt_d[l, 8 + dc], 1024)
                    pya, pyb, pga, pgb = gp.next(), gp.next(), gp.next(), gp.next()
                    for kc in range(4):
                        mm(pya[:, :], wbm[:, kc * 128:(kc + 1) * 128], otmT[:, kc * 512:(kc + 1) * 512], kc == 0, kc == 3, [wbm, otmT], [pya])
                    for kc in range(8):
                        mm(pyb[:, :], wbd[:, kc * 128:(kc + 1) * 128], otdT[:, kc * 512:(kc + 1) * 512], kc == 0, kc == 7, [wbd, otdT], [pyb])
                    for kc in range(8):
                        mm(pga[:, :], wga[:, kc * 128:(kc + 1) * 128], hT[kc][tb][:, :], kc == 0, kc == 7, [wga, hT[kc][tb]], [pga])
                    for kc in range(8):
                        mm(pgb[:, :], wgb[:, kc * 128:(kc + 1) * 128], hT[kc][tb][:, :], kc == 0, kc == 7, [wgb, hT[kc][tb]], [pgb])
                    sa, sb_ = tmpA.next(), tmpA.next()
                    act(sa[:, :], pga[:, :], AF.Sigmoid, [pga], [sa])
                    act(sb_[:, :], pgb[:, :], AF.Sigmoid, [pgb], [sb_])
                    dve(lambda: nc.vector.tensor_tensor(sa[:, :], sa[:, :], pya[:, :], ALU.mult), [sa, pya], [sa])
                    dve(lambda: nc.vector.tensor_tensor(sb_[:, :], sb_[:, :], pyb[:, :], ALU.mult), [sb_, pyb], [sb_])
                    dve(lambda: nc.vector.tensor_tensor(mrg[dc][:, :], sa[:, :], sb_[:, :], ALU.add), [sa, sb_], [mrg[dc]])
                for dc in range(8):
                    wo = wload(w_out_d[l, dc], 1024)
                    p = gp.next()
                    for kc in range(8):
                        mm(p[:, :], wo[:, kc * 128:(kc + 1) * 128], mrg[kc][:, :], kc == 0, kc == 7, [wo, mrg[kc]], [p])
                    x_ = xT[dc][tb]
                    dve(lambda: nc.vector.scalar_tensor_tensor(x_[:, :], p[:, :], dvcol(l, s, G1, dc), x_[:, :], ALU.mult, ALU.add),
                        [p, dvt, x_], [x_])
                layernorm(tb, "ln1_g", "ln1_b", l, GA2, GB2, s, True)

            if stop == 'merge':
                raise Stop()
            rd = inherit([otmT, otdT] + mrg)
            gTt = T(arena[:, 0:ARENA_N], "gT", rd)
            if is_moe:
                li = l // 2
            nexp = NEXP if is_moe else 1
            for tbb in range(2):
                if is_moe:
                    for t8 in range(8):
                        tb, off = (tbb * 8 + t8) // 4, ((tbb * 8 + t8) % 4) * 128
                        p = gp.next()
                        for kc in range(8):
                            mm(p[:, 0:8], hT[kc][tb][:, off:off + 128], rwb[li][:, kc * 8:(kc + 1) * 8], kc == 0, kc == 7, [hT[kc][tb], rwb[li]], [p])
                        lg = lgt.next()
                        rbv = vecs[:, V("rb") + li * 8: V("rb") + li * 8 + 8]
                        dve(lambda: nc.vector.tensor_tensor(lg[:, 0:8], p[:, 0:8], rbv, ALU.add), [p, vecs], [lg])
                        dve(lambda: nc.vector.reduce_max(lg[:, 32:33], lg[:, 0:8], axis=AX.X), [lg], [lg])
                        dve(lambda: nc.vector.tensor_scalar(lg[:, 8:16], lg[:, 0:8], lg[:, 32:33], None, ALU.is_equal), [lg], [lg])
                        dve(lambda: nc.vector.scalar_tensor_tensor(lg[:, 16:24], lg[:, 8:16], -1e30, lg[:, 0:8], ALU.mult, ALU.add), [lg], [lg])
                        dve(lambda: nc.vector.reduce_max(lg[:, 33:34], lg[:, 16:24], axis=AX.X), [lg], [lg])
                        dve(lambda: nc.vector.tensor_scalar(lg[:, 24:32], lg[:, 16:24], lg[:, 33:34], None, ALU.is_equal), [lg], [lg])
                        dve(lambda: nc.vector.tensor_tensor(lg[:, 34:35], lg[:, 32:33], lg[:, 33:34], ALU.subtract), [lg], [lg])
                        act(lg[:, 35:36], lg[:, 34:35], AF.Sigmoid, [lg], [lg])
                        act(lg[:, 36:37], lg[:, 34:35], AF.Sigmoid, [lg], [lg], scale=-1.0)
                        dve(lambda: nc.vector.tensor_scalar(lg[:, 8:16], lg[:, 8:16], lg[:, 35:36], None, ALU.mult), [lg], [lg])
                        dve(lambda: nc.vector.scalar_tensor_tensor(lg[:, 40:48], lg[:, 24:32], lg[:, 36:37], lg[:, 8:16], ALU.mult, ALU.add), [lg], [lg])
                        dve(lambda: nc.vector.tensor_copy(lgb[:, 0:8], lg[:, 40:48]), [lg], [lgb])
                        E.op("pe", lambda: nc.tensor.transpose(ptb[0:8, 0:128], lgb[:, 0:8], ident_bf[:, :]), [lgb, ident_bf], [ptb])
                        dve(lambda: nc.vector.tensor_copy(GT[0:8, t8 * 128:(t8 + 1) * 128], ptb[0:8, 0:128]), [ptb], [GT])
                for e in range(nexp):
                    ei = eidx(l, e)
                    if is_moe:
                        g_ = gb.next()
                        for half in range(2):
                            p = gp.next()
                            mm(p[:, :], sel[0:8, e * 128:(e + 1) * 128], GT[0:8, half * 512:(half + 1) * 512], True, True, [sel, GT], [p])
                            act(g_[:, half * 512:(half + 1) * 512], p[:, :], AF.Copy, [p], [g_])
                    for fc in range(NFC):
                        w1 = wload(w1_d[ei, fc], 1024)
                        w3 = wload(w3_d[ei, fc], 1024)
                        for half in range(2):
                            tb = tbb * 2 + half
                            pa = gp.next()
                            pb_ = gp.next()
                            for kc in range(8):
                                mm(pa[:, :], w1[:, kc * 128:(kc + 1) * 128], hT[kc][tb][:, :], kc == 0, kc == 7, [w1, hT[kc][tb]], [pa])
                            for kc in range(8):
                                mm(pb_[:, :], w3[:, kc * 128:(kc + 1) * 128], hT[kc][tb][:, :], kc == 0, kc == 7, [w3, hT[kc][tb]], [pb_])
                            sa = tmpB.next()
                            act(sa[:, :], pa[:, :], AF.Silu, [pa], [sa])
                            gsl = gTt[:, fc * 1024 + half * 512: fc * 1024 + (half + 1) * 512]
                            if is_moe:
                                sa2 = tmpB.next()
                                dve(lambda: nc.vector.tensor_tensor(sa2[:, :], sa[:, :], pb_[:, :], ALU.mult), [sa, pb_], [sa2])
                                dve(lambda: nc.vector.tensor_tensor(gsl, sa2[:, :], g_[:, half * 512:(half + 1) * 512], ALU.mult), [sa2, g_], [gTt])
                            else:
                                dve(lambda: nc.vector.tensor_tensor(gsl, sa[:, :], pb_[:, :], ALU.mult), [sa, pb_], [gTt])
                    for dc in range(8):
                        w2h = [wload(w2_d[ei, dc, 0], 1024), wload(w2_d[ei, dc, 1], 1024), wload(w2_d[ei, dc, 2][:, 0:768], 768)]
                        for half in range(2):
                            tb = tbb * 2 + half
                            p = gp.next()
                            for fc in range(NFC):
                                w2 = w2h[fc // 8]
                                mm(p[:, :], w2[:, (fc % 8) * 128:(fc % 8 + 1) * 128], gTt[:, fc * 1024 + half * 512: fc * 1024 + (half + 1) * 512],
                                   fc == 0, fc == NFC - 1, [w2, gTt], [p])
                            x_ = xT[dc][tb]
                            dve(lambda: nc.vector.scalar_tensor_tensor(x_[:, :], p[:, :], dvcol(l, s, G2, dc), x_[:, :], ALU.mult, ALU.add),
                                [p, dvt, x_], [x_])
                for half in range(2):
                    layernorm(tbb * 2 + half, "ln2_g", "ln2_b", l, GAN, GBN, s, l < n_layers - 1)
            arena_prev = [gTt]


    for s in range(n_seq):
        try:
            run_seq(s)
        except Stop:
            pass
        for c in range(8):
            for tb in range(4):
                E.dma("sp", outT_d[s, c * 128:(c + 1) * 128, tb * 512:(tb + 1) * 512], xT[c][tb][:, :],
                      reads=[xT[c][tb]], writes=[Tout[s]])
    E.finish(Tout)
    return nc, em


def tile_w(W, B):
    K, N = W.shape
    KC = K // 128
    NB = N // B
    return np.ascontiguousarray(W.reshape(KC, 128, NB, B).transpose(2, 1, 0, 3)).reshape(NB, 128, KC * B)


def prep_shared(inp):
    f = lambda a: np.asarray(a, dtype=np.float32)
    w_in = f(inp["w_in"])
    o = {}
    o["w_ada"] = np.stack([tile_w(f(inp["w_ada"][l]), 128) for l in range(L)])
    o["w_lat"] = np.stack([tile_w(w_in[l][:, 0:640], 128) for l in range(L)])
    z64 = np.zeros((D, 64), np.float32)
    kr = []
    for l in range(L):
        r = w_in[l][:, 640:672]
        A = np.concatenate([z64, r], axis=1)
        B = np.concatenate([z64, r[:, 16:32], r[:, 0:16]], axis=1)
        kr.append(np.stack([tile_w(A, 96)[0], tile_w(B, 96)[0]]))
    o["w_kr"] = np.stack(kr)
    c0 = 672
    o["w_dq"] = np.stack([tile_w(w_in[l][:, c0:c0 + 1024], 128) for l in range(L)])
    o["w_dk"] = np.stack([tile_w(w_in[l][:, c0 + 1024:c0 + 2048], 128) for l in range(L)])
    o["w_dv"] = np.stack([tile_w(w_in[l][:, c0 + 2048:c0 + 3072], 128) for l in range(L)])
    o["w_gt"] = np.stack([tile_w(w_in[l][:, c0 + 3072:c0 + 5120], 128) for l in range(L)])
    wq = f(inp["w_q_up"])
    qs = []
    for l in range(L):
        hs = []
        for h in range(8):
            Wh = wq[l][:, h * 96:(h + 1) * 96]
            A = Wh
            B = np.concatenate([np.zeros((384, 64), np.float32), Wh[:, 80:96], Wh[:, 64:80]], axis=1)
            hs.append(np.stack([tile_w(A, 96)[0], tile_w(B, 96)[0]]))
        qs.append(np.stack(hs))
    o["w_q"] = np.stack(qs)
    wkv = f(inp["w_kv_up"])
    o["w_kn"] = np.stack([np.stack([tile_w(wkv[l][:, h * 128:h * 128 + 64], 64)[0] for h in range(8)]) for l in range(L)])
    o["w_v"] = np.stack([np.stack([tile_w(wkv[l][:, h * 128 + 64:h * 128 + 128], 64)[0] for h in range(8)]) for l in range(L)])
    o["w_brm"] = np.stack([tile_w(f(inp["w_br_mla"][l]), 128) for l in range(L)])
    o["w_brd"] = np.stack([tile_w(f(inp["w_br_diff"][l]), 128) for l in range(L)])
    o["w_out"] = np.stack([tile_w(f(inp["w_out"][l]), 128) for l in range(L)])
    w1l, w3l, w2l = [], [], []
    fw1, fw3, fw2 = f(inp["ffn_w1"]), f(inp["ffn_w3"]), f(inp["ffn_w2"])
    mw1, mw3, mw2 = f(inp["moe_w1"]), f(inp["moe_w3"]), f(inp["moe_w2"])
    for i in range(2):
        w1l.append(tile_w(fw1[i], 128)); w3l.append(tile_w(fw3[i], 128)); w2l.append(tile_w(fw2[i], 128))
    for i in range(2):
        for e in range(NEXP):
            w1l.append(tile_w(mw1[i, e], 128)); w3l.append(tile_w(mw3[i, e], 128)); w2l.append(tile_w(mw2[i, e], 128))
    o["w1"] = np.stack(w1l)
    o["w3"] = np.stack(w3l)
    w2a = np.stack(w2l)
    w2p = np.zeros((NE, 8, 128, 24 * 128), np.float32)
    w2p[..., :NFC * 128] = w2a
    o["w2"] = np.ascontiguousarray(w2p.reshape(NE, 8, 128, 3, 1024).transpose(0, 1, 3, 2, 4))
    o["ident"] = np.eye(128, dtype=np.float32)
    o["rw"] = np.stack([tile_w(f(inp["router_w"][i]), 8)[0] for i in range(2)])
    vec = np.zeros((128, NV), np.float32)

    def put(name, arr):
        arr = np.asarray(arr, np.float32)
        vec[:, VL[name]:VL[name] + arr.shape[1]] = arr
    put("b_ada", f(inp["b_ada"]).reshape(L, 48, 128).transpose(2, 0, 1).reshape(128, L * 48))
    for nm, key in (("ln1_g", "ln1_g"), ("ln1_b", "ln1_b"), ("ln2_g", "ln2_g"), ("ln2_b", "ln2_b")):
        put(nm, f(inp[key]).reshape(L, 8, 128).transpose(2, 0, 1).reshape(128, L * 8))
    put("qg", f(inp["q_norm_g"]).reshape(L, 3, 128).transpose(2, 0, 1).reshape(128, L * 3))
    put("kvg", f(inp["kv_norm_g"]).reshape(L, 2, 128).transpose(2, 0, 1).reshape(128, L * 2))
    put("dng", f(inp["diff_norm_g"]).reshape(L, 8, 128).transpose(2, 0, 1).reshape(128, L * 8))
    lam = np.stack([f(inp["lambda_q1"]), f(inp["lambda_k1"]), f(inp["lambda_q2"]), f(inp["lambda_k2"])], axis=1)
    o["lam"] = np.ascontiguousarray(np.broadcast_to(lam.reshape(1, L * 256), (128, L * 256)))
    put("rb", np.broadcast_to(f(inp["router_b"]).reshape(1, 16), (128, 16)))
    inv_freq = (np.float32(10000.0) ** (-np.arange(0, 32, 2, dtype=np.float32) / np.float32(32))).astype(np.float32)
    fr = np.zeros((128, 1), np.float32)
    fr[64:80, 0] = inv_freq
    fr[80:96, 0] = inv_freq
    put("freq", fr)
    ns = np.zeros((128, 1), np.float32)
    ns[64:80, 0] = -1.0
    ns[80:96, 0] = 1.0
    put("nsgn", ns)
    o["vecs"] = vec
    sel = np.zeros((8, 8, 128), np.float32)
    for e in range(8):
        sel[e, e, :] = 1.0
    o["sel"] = sel.reshape(8, 1024)
    return o


def prep_core(inp, core):
    x = np.asarray(inp["x"], np.float32)[core * NS:(core + 1) * NS]
    c = np.asarray(inp["c"], np.float32)[core * NS:(core + 1) * NS]
    pos = np.asarray(inp["positions"], np.int32)[core * NS:(core + 1) * NS]
    o = {}
    o["xT"] = np.ascontiguousarray(x.transpose(0, 2, 1))
    o["posb"] = np.ascontiguousarray(np.broadcast_to(pos[:, None, :], (NS, 128, S)))
    o["posk"] = np.ascontiguousarray(pos.reshape(NS, 16, 128).transpose(0, 2, 1))
    o["cT"] = np.ascontiguousarray(c.reshape(NS, 8, 128).transpose(2, 1, 0)).reshape(128, 8 * NS)
    return o


_CACHE = {}


def kernel(**inputs):
    n = 8
    shared = prep_shared(inputs)
    if "nc" not in _CACHE:
        _CACHE["nc"] = build()[0]
    nc = _CACHE["nc"]
    in_maps = []
    for core in range(n):
        m = dict(shared)
        m.update(prep_core(inputs, core))
        in_maps.append(m)
    res = run_bass_kernel_spmd(nc, in_maps, core_ids=list(range(n)))
    outs = [np.asarray(r["outT"]).transpose(0, 2, 1) for r in res.results]
    return np.ascontiguousarray(np.concatenate(outs, axis=0)).astype(np.float32)
```

```python
import math
import numpy as np
from contextlib import ExitStack
import concourse.bass as bass
import concourse.mybir as mybir
from concourse.bass_utils import run_bass_kernel_spmd

F32 = mybir.dt.float32
BF16 = mybir.dt.bfloat16
I32 = mybir.dt.int32
AF = mybir.ActivationFunctionType
ALU = mybir.AluOpType
AX = mybir.AxisListType

L = 4
D = 1024
S = 2048
NS = 2
F = 2816
NFC = 22
NEXP = 8
NE = 2 + 2 * NEXP
ALPHA = (2.0 * L) ** 0.25
LN_EPS = 1e-5
RMS_EPS = 1e-6
MLA_SCALE = 96.0 ** -0.5
DIFF_SCALE = 64.0 ** -0.5
TWO_PI = 2.0 * math.pi


class T:
    __slots__ = ("ap", "name", "last_w", "readers")

    def __init__(self, ap, name="", readers=None):
        self.ap = ap
        self.name = name
        self.last_w = None
        self.readers = dict(readers) if readers else {}

    def __getitem__(self, idx):
        return self.ap[idx]


def inherit(tiles):
    r = {}
    for t in tiles:
        for k, v in t.readers.items():
            r[k] = max(r.get(k, 0), v)
        if t.last_w is not None:
            k, v = t.last_w
            r[k] = max(r.get(k, 0), v)
    return r


class Em:
    NRING = 16
    SEM_LIMIT = 30000

    def __init__(self, nc):
        self.nc = nc
        self.eng = {"pe": nc.tensor, "act": nc.scalar, "dve": nc.vector,
                    "pool": nc.gpsimd, "sp": nc.sync}
        self.sem = {}
        self.cnt = {}
        self.known = {e: {} for e in self.eng}
        self.cur = {}
        self.nep = {}
        for e in self.eng:
            self.sem[e] = nc.alloc_semaphore(name="s_" + e)
            self.cnt[e] = 0
            self.cur[e] = e
            self.nep[e] = 0
        self.ring = {}
        for q in ("sp", "pool"):
            lst = []
            for i in range(self.NRING):
                key = "r_%s%d" % (q, i)
                self.sem[key] = nc.alloc_semaphore(name=key)
                self.cnt[key] = 0
                lst.append(key)
            self.ring[q] = [lst, 0]
        self.n_ins = {e: 0 for e in self.eng}
        self.n_wait = {e: 0 for e in self.eng}

    def _need(self, e, ev):
        if ev is None:
            return
        k, v = ev
        if e == "pe" and k.split("#")[0] == "pe":
            return
        if self.known[e].get(k, 0) >= v:
            return
        self.eng[e].wait_ge(self.sem[k], v)
        self.n_wait[e] += 1
        self.known[e][k] = v

    def _deps(self, e, reads, writes):
        for t in reads:
            self._need(e, t.last_w)
        for t in writes:
            self._need(e, t.last_w)
            for k, v in list(t.readers.items()):
                self._need(e, (k, v))

    def _commit(self, ev, reads, writes):
        k, v = ev
        for t in reads:
            if t.readers.get(k, 0) < v:
                t.readers[k] = v
        for t in writes:
            t.last_w = ev
            t.readers = {}

    def op(self, e, fn, reads=(), writes=()):
        self._deps(e, reads, writes)
        ins = fn()
        if e == "pe" and len(reads) > 0 and reads[0].last_w is not None and reads[0].last_w[0].split("#")[0] != "pe":
            k0, v0 = reads[0].last_w
            ins._wait_ge(self.sem[k0], v0)
        key = self.cur[e]
        if self.cnt[key] >= self.SEM_LIMIT:
            self.nep[e] += 1
            key = "%s#%d" % (e, self.nep[e])
            self.sem[key] = self.nc.alloc_semaphore(name="s_%s_%d" % (e, self.nep[e]))
            self.cnt[key] = 0
            self.cur[e] = key
        self.cnt[key] += 1
        ins.then_inc(self.sem[key], 1)
        ev = (key, self.cnt[key])
        self._commit(ev, reads, writes)
        self.n_ins[e] += 1
        return ev

    def dma(self, q, out_ap, in_ap, reads=(), writes=(), **kw):
        self._deps(q, reads, writes)
        lst, pos = self.ring[q]
        key = lst[pos % self.NRING]
        self.ring[q][1] = pos + 1
        if self.cnt[key] > 0:
            self._need(q, (key, self.cnt[key]))
        ins = self.eng[q].dma_start(out=out_ap, in_=in_ap, **kw)
        self.cnt[key] += 16
        ins.then_inc(self.sem[key], 16)
        ev = (key, self.cnt[key])
        self._commit(ev, reads, writes)
        self.n_ins[q] += 1
        return ev

    def finish(self, tiles):
        for t in tiles:
            self._need("sp", t.last_w)
        for e in self.eng:
            key = self.cur[e]
            if e != "sp" and self.cnt[key] > 0:
                self._need("sp", (key, self.cnt[key]))


class Rot:
    def __init__(self, items):
        self.items = items
        self.i = 0

    def next(self):
        t = self.items[self.i % len(self.items)]
        self.i += 1
        return t


def vlay():
    lay = {}
    off = 0
    for name, w in (("b_ada", L * 48), ("ln1_g", L * 8), ("ln1_b", L * 8), ("ln2_g", L * 8),
                    ("ln2_b", L * 8), ("qg", L * 3), ("kvg", L * 2), ("dng", L * 8),
                    ("rb", 2 * 8), ("freq", 1), ("nsgn", 1)):
        lay[name] = off
        off += w
    return lay, off


VL, NV = vlay()


def eidx(l, e):
    return (l // 2) if l % 2 == 0 else 2 + (l // 2) * NEXP + e


class Stop(Exception):
    pass


def build(n_layers=L, n_seq=NS, stop=None):
    nc = bass.Bass("TRN2", target_bir_lowering=False)
    es = ExitStack()

    def din(name, shape, dt=F32):
        return nc.dram_tensor(name, list(shape), dt, kind="ExternalInput").ap()

    Ld = n_layers
    NEd = max(eidx(l, e) for l in range(n_layers) for e in range(NEXP if l % 2 else 1)) + 1
    xT_d = din("xT", [NS, D, S])
    posb_d = din("posb", [NS, 128, S], I32)
    posk_d = din("posk", [NS, 128, 16], I32)
    cT_d = din("cT", [128, 8 * NS])
    vecs_d = din("vecs", [128, NV])
    sel_d = din("sel", [8, 8 * 128])
    ident_d = din("ident", [128, 128])
    lam_d = din("lam", [128, L * 256])
    w_ada_d = din("w_ada", [Ld, 48, 128, 1024])
    w_lat_d = din("w_lat", [Ld, 5, 128, 1024])
    w_kr_d = din("w_kr", [Ld, 2, 128, 8 * 96])
    w_dq_d = din("w_dq", [Ld, 8, 128, 1024])
    w_dk_d = din("w_dk", [Ld, 8, 128, 1024])
    w_dv_d = din("w_dv", [Ld, 8, 128, 1024])
    w_gt_d = din("w_gt", [Ld, 16, 128, 1024])
    w_q_d = din("w_q", [Ld, 8, 2, 128, 3 * 96])
    w_kn_d = din("w_kn", [Ld, 8, 128, 128])
    w_v_d = din("w_v", [Ld, 8, 128, 128])
    w_brm_d = din("w_brm", [Ld, 8, 128, 512])
    w_brd_d = din("w_brd", [Ld, 8, 128, 1024])
    w_out_d = din("w_out", [Ld, 8, 128, 1024])
    w1_d = din("w1", [NEd, NFC, 128, 1024])
    w3_d = din("w3", [NEd, NFC, 128, 1024])
    w2_d = din("w2", [NEd, 8, 3, 128, 1024])
    rw_d = din("rw", [2, 128, 64])
    outT_d = nc.dram_tensor("outT", [NS, D, S], F32, kind="ExternalOutput").ap()
    etab_d = nc.dram_tensor("etab", [NS, 8, S, S], BF16).ap()
    otm_d = nc.dram_tensor("otm", [512, S], BF16).ap()
    otd_d = nc.dram_tensor("otd", [1024, S], BF16).ap()

    em = Em(nc)
    E = em

    def sbt(name, shape, dt):
        return es.enter_context(nc.sbuf_tensor("sb_" + name, list(shape), dt))

    def sb(name, shape, dt):
        return T(sbt(name, shape, dt), name)

    def pst(name, shape, dt):
        return es.enter_context(nc.psum_tensor("ps_" + name, list(shape), dt))

    Din = T(None, "dram_in")
    Tout = [T(None, "out%d" % s) for s in range(NS)]
    Tetab = [[T(None, "etab") for h in range(8)] for s in range(NS)]
    Totm = [T(None, "otm%d" % tb) for tb in range(4)]
    Totd = [T(None, "otd%d" % tb) for tb in range(4)]

    xT_t = sbt("xT_t", [128, 8 * S], F32)
    hT_t = sbt("hT_t", [128, 8 * S], BF16)
    xT = [[T(xT_t[:, c * S + tb * 512: c * S + (tb + 1) * 512], "x%d_%d" % (c, tb)) for tb in range(4)]
          for c in range(8)]
    hT = [[T(hT_t[:, c * S + tb * 512: c * S + (tb + 1) * 512], "h%d_%d" % (c, tb)) for tb in range(4)]
          for c in range(8)]
    vecs = sb("vecs", [128, NV], F32)
    cT = sb("cT", [128, 8 * NS], F32)
    cond = sb("cond", [128, 8 * NS], F32)
    mod = sb("mod", [128, L * 48 * NS], F32)
    NDV = 8
    dvt = sb("dvt", [128, L * NS * NDV * 8], F32)
    nlam = sb("nlam", [128, L], F32)
    lamtmp = sb("lamtmp", [128, 64 + 4], F32)
    ones_bf = sb("ones_bf", [128, 128], BF16)
    ones_f = sb("ones_f", [128, 128], F32)
    ident_bf = sb("ident_bf", [128, 128], BF16)
    ident_f = sb("ident_f", [128, 128], F32)
    sel = sb("sel", [8, 8 * 128], BF16)
    rw = [sb("rw%d" % i, [128, 64], F32) for i in range(2)]
    rwp = sb("rwp", [128, 64], F32)
    brow = sb("brow", [1, 8], F32)
    tabC = sb("tabC", [128, S], BF16)
    tabS = sb("tabS", [128, S], BF16)
    posk_i = sb("posk_i", [128, 16], I32)
    posk = sb("posk", [128, 16], F32)
    NWS = 8
    wslots = Rot([sb("wsl%d" % i, [128, 1024], BF16) for i in range(NWS)])
    wstage = Rot([sb("wstg%d" % i, [128, 1024], F32) for i in range(2)])
    PTs = Rot([sb("PT%d" % i, [128, 512], BF16) for i in range(6)])
    Ets = Rot([sb("Et%d" % i, [128, 512], BF16) for i in range(3)])
    OTst = Rot([sb("OTst%d" % i, [128, 512], BF16) for i in range(2)])
    tmpA = Rot([sb("tmpA%d" % i, [128, 512], F32) for i in range(3)])
    tmpB = Rot([sb("tmpB%d" % i, [128, 512], BF16) for i in range(3)])
    small = Rot([sb("small%d" % i, [128, 16], F32) for i in range(6)])
    lgt = Rot([sb("lgt%d" % i, [128, 64], F32) for i in range(2)])
    GT = sb("GT", [8, 1024], BF16)
    gb = Rot([sb("gb%d" % i, [128, 1024], BF16) for i in range(1)])
    ARENA_N = 22560
    arena = sbt("arena", [128, ARENA_N], BF16)

    pb = [T(pst("pb%d" % i, [128, 512], F32), "pb%d" % i) for i in range(7)]
    ptb = T(pst("ptb", [128, 1024], BF16), "ptb")
    gp = Rot(pb[0:4])
    gp7 = Rot(pb)

    def mm(out_ap, lhsT, rhs, start, stop, reads, writes, **kw):
        E.op("pe", lambda: nc.tensor.matmul(out_ap, lhsT, rhs, start=start, stop=stop, **kw), reads, writes)

    def act(out_ap, in_ap, func, reads, writes, **kw):
        E.op("act", lambda: nc.scalar.activation(out_ap, in_ap, func, **kw), reads, writes)

    def dve(fn, reads, writes):
        E.op("dve", fn, reads, writes)

    def V(name):
        return VL[name]

    def vcol(name, i):
        o = VL[name] + i
        return vecs[:, o:o + 1]

    wcnt = [0]

    def wload(dram_ap, n, reads=(Din,)):
        st_ = wstage.next()
        E.dma("sp", st_[:, 0:n], dram_ap, reads=list(reads), writes=[st_])
        w = wslots.next()
        wcnt[0] += 1
        if wcnt[0] % 2 == 0:
            dve(lambda: nc.vector.tensor_copy(w[:, 0:n], st_[:, 0:n]), [st_], [w])
        else:
            act(w[:, 0:n], st_[:, 0:n], AF.Copy, [st_], [w])
        return w

    def dvcol(l, s, k, c):
        o = ((l * NS + s) * NDV + k) * 8 + c
        return dvt[:, o:o + 1]

    def dvvec(l, s, k):
        o = ((l * NS + s) * NDV + k) * 8
        return dvt[:, o:o + 8]

    def modvec(l, blk, s):
        o = (l * 6 + blk) * 8 * NS
        return mod[:, o:o + 8 * NS].rearrange("p (m s) -> p m s", s=NS)[:, :, s]

    def modcol(l, blk, s, c):
        o = ((l * 6 + blk) * 8 + c) * NS + s
        return mod[:, o:o + 1]

    A1, G1, A2, G2, GA2, GB2, GAN, GBN = range(8)

    E.dma("sp", vecs[:, :], vecs_d[:, :], reads=[Din], writes=[vecs])
    E.dma("sp", cT[:, :], cT_d[:, :], reads=[Din], writes=[cT])
    self_ = wstage.next()
    E.dma("sp", self_[0:8, :], sel_d[:, :], reads=[Din], writes=[self_])
    dve(lambda: nc.vector.tensor_copy(sel[:, :], self_[0:8, :]), [self_], [sel])
    E.dma("sp", ident_f[:, :], ident_d[:, :], reads=[Din], writes=[ident_f])
    for i in range(2):
        E.dma("sp", rw[i][:, :], rw_d[i], reads=[Din], writes=[rw[i]])
    dve(lambda: nc.vector.memset(ones_bf[:, :], 1.0), [], [ones_bf])
    dve(lambda: nc.vector.memset(ones_f[:, :], 1.0), [], [ones_f])
    dve(lambda: nc.vector.tensor_copy(ident_bf[:, :], ident_f[:, :]), [ident_f], [ident_bf])
    act(cond[:, :], cT[:, :], AF.Silu, [cT], [cond])
    cond_bf = sb("cond_bf", [128, 8 * NS], BF16)
    dve(lambda: nc.vector.tensor_copy(cond_bf[:, :], cond[:, :]), [cond], [cond_bf])
    rwb = [sb("rwb%d" % i, [128, 64], BF16) for i in range(2)]
    for i in range(2):
        dve(lambda: nc.vector.tensor_copy(rwb[i][:, :], rw[i][:, :]), [rw[i]], [rwb[i]])
    lgb = sb("lgb", [128, 8], BF16)

    for l in range(n_layers):
        for blk in range(6):
            for m in range(8):
                w = wload(w_ada_d[l, blk * 8 + m], 1024)
                p = gp.next()
                for kc in range(8):
                    mm(p[:, 0:NS], w[:, kc * 128:(kc + 1) * 128], cond_bf[:, kc * NS:(kc + 1) * NS],
                       kc == 0, kc == 7, [w, cond_bf], [p])
                o = ((l * 6 + blk) * 8 + m) * NS
                dve(lambda: nc.vector.tensor_scalar(mod[:, o:o + NS], p[:, 0:NS], vcol("b_ada", l * 48 + blk * 8 + m),
                                                    None, ALU.add), [p, vecs], [mod])
    for l in range(n_layers):
        for s in range(n_seq):
            dve(lambda: nc.vector.tensor_scalar(dvvec(l, s, A1), modvec(l, 1, s), 1.0, None, ALU.add), [mod], [dvt])
            dve(lambda: nc.vector.tensor_scalar(dvvec(l, s, G1), modvec(l, 2, s), 1.0 / ALPHA, None, ALU.mult), [mod], [dvt])
            dve(lambda: nc.vector.tensor_scalar(dvvec(l, s, A2), modvec(l, 4, s), 1.0, None, ALU.add), [mod], [dvt])
            dve(lambda: nc.vector.tensor_scalar(dvvec(l, s, G2), modvec(l, 5, s), 1.0 / ALPHA, None, ALU.mult), [mod], [dvt])
            g1v = vecs[:, V("ln1_g") + l * 8: V("ln1_g") + l * 8 + 8]
            b1v = vecs[:, V("ln1_b") + l * 8: V("ln1_b") + l * 8 + 8]
            dve(lambda: nc.vector.tensor_tensor(dvvec(l, s, GA2), g1v, dvvec(l, s, A2), ALU.mult), [vecs, dvt], [dvt])
            dve(lambda: nc.vector.tensor_tensor(dvvec(l, s, GB2), b1v, dvvec(l, s, A2), ALU.mult), [vecs, dvt], [dvt])
            dve(lambda: nc.vector.tensor_tensor(dvvec(l, s, GB2), dvvec(l, s, GB2), modvec(l, 3, s), ALU.add), [mod, dvt], [dvt])
    for l in range(n_layers - 1):
        for s in range(n_seq):
            g2v = vecs[:, V("ln2_g") + l * 8: V("ln2_g") + l * 8 + 8]
            b2v = vecs[:, V("ln2_b") + l * 8: V("ln2_b") + l * 8 + 8]
            dve(lambda: nc.vector.tensor_tensor(dvvec(l, s, GAN), g2v, dvvec(l + 1, s, A1), ALU.mult), [vecs, dvt], [dvt])
            dve(lambda: nc.vector.tensor_tensor(dvvec(l, s, GBN), b2v, dvvec(l + 1, s, A1), ALU.mult), [vecs, dvt], [dvt])
            dve(lambda: nc.vector.tensor_tensor(dvvec(l, s, GBN), dvvec(l, s, GBN), modvec(l + 1, 0, s), ALU.add), [mod, dvt], [dvt])
    lamv = T(arena[:, 8192:8192 + 2 * L * 256].bitcast(F32), "lamv")
    E.dma("sp", lamv[:, :], lam_d[:, :], reads=[Din], writes=[lamv])
    for l in range(n_layers):
        lam_init = 0.8 - 0.6 * math.exp(-0.3 * l)
        lo = l * 256
        for j in range(2):
            dve(lambda: nc.vector.tensor_tensor(lamtmp[:, 0:64], lamv[:, lo + j * 128: lo + j * 128 + 64],
                                                lamv[:, lo + j * 128 + 64: lo + j * 128 + 128], ALU.mult), [lamv], [lamtmp])
            dve(lambda: nc.vector.reduce_sum(lamtmp[:, 64 + j:65 + j], lamtmp[:, 0:64], axis=AX.X), [lamtmp], [lamtmp])
            act(lamtmp[:, 66 + j:67 + j], lamtmp[:, 64 + j:65 + j], AF.Exp, [lamtmp], [lamtmp])
        dve(lambda: nc.vector.tensor_tensor(lamtmp[:, 64:65], lamtmp[:, 67:68], lamtmp[:, 66:67], ALU.subtract), [lamtmp], [lamtmp])
        dve(lambda: nc.vector.tensor_scalar(nlam[:, l:l + 1], lamtmp[:, 64:65], -lam_init, None, ALU.add), [lamtmp], [nlam])

    arena_prev = [lamv]
    hprev = []

    def layernorm(tb, lng, lnb, l, GAk, GBk, s, write_h):
        psum_s = pb[4]
        psum_q = pb[5]
        for dc in range(8):
            ub = tmpB.next()
            act(ub[:, :], xT[dc][tb][:, :], AF.Copy, [xT[dc][tb]], [ub])
            mm(psum_s[:, :], ones_bf[:, :], ub[:, :], dc == 0, dc == 7, [ones_bf, ub], [psum_s])
            uq = tmpB.next()
            act(uq[:, :], xT[dc][tb][:, :], AF.Square, [xT[dc][tb]], [uq])
            mm(psum_q[:, :], ones_bf[:, :], uq[:, :], dc == 0, dc == 7, [ones_bf, uq], [psum_q])
        mean = tmpA.next()
        dve(lambda: nc.vector.tensor_scalar(mean[:, :], psum_s[:, :], 1.0 / D, None, ALU.mult), [psum_s], [mean])
        msq = tmpA.next()
        dve(lambda: nc.vector.tensor_tensor(msq[:, :], mean[:, :], mean[:, :], ALU.mult), [mean], [msq])
        dve(lambda: nc.vector.scalar_tensor_tensor(msq[:, :], psum_q[:, :], 1.0 / D, msq[:, :], ALU.mult, ALU.subtract),
            [psum_q, msq], [msq])
        act(msq[:, :], msq[:, :], AF.Sqrt, [msq], [msq], bias=LN_EPS / (ALPHA * ALPHA), scale=1.0)
        rstd = tmpA.next()
        dve(lambda: nc.vector.reciprocal(rstd[:, :], msq[:, :]), [msq], [rstd])
        for dc in range(8):
            x_ = xT[dc][tb]
            dve(lambda: nc.vector.tensor_tensor(x_[:, :], x_[:, :], mean[:, :], ALU.subtract), [x_, mean], [x_])
            dve(lambda: nc.vector.tensor_tensor(x_[:, :], x_[:, :], rstd[:, :], ALU.mult), [x_, rstd], [x_])
            if write_h:
                h_ = hT[dc][tb]
                dve(lambda: nc.vector.tensor_scalar(h_[:, :], x_[:, :], dvcol(l, s, GAk, dc), dvcol(l, s, GBk, dc),
                                                    ALU.mult, ALU.add), [x_, dvt], [h_])
            dve(lambda: nc.vector.tensor_scalar(x_[:, :], x_[:, :], vcol(lng, l * 8 + dc), vcol(lnb, l * 8 + dc),
                                                ALU.mult, ALU.add), [x_, vecs], [x_])

    gps = Rot(pb[0:3])
    gps_mla = Rot([pb[0], pb[1], pb[2], pb[5], pb[6]])

    def attention(QT, KT, Vt, krows, scale, nmaps, dv, s, h, finish_fn):
        Vv = Vt[:, 0:16 * dv].rearrange("p (t d) -> p t d", d=dv)
        accO = [pb[3], pb[5]]
        accS = [pb[4], pb[6]]
        sbanks = gps if nmaps == 2 else gps_mla

        def scores(qb, kt):
            qs = slice(qb * 512, (qb + 1) * 512)
            ks = slice(kt * 128, (kt + 1) * 128)
            PTl = []
            if nmaps == 2:
                et = Ets.next()
                E.dma("sp", et[:, :], etab_d[s, h, kt * 128:(kt + 1) * 128, qs], reads=[Tetab[s][h]], writes=[et])
            for m in range(nmaps):
                p = sbanks.next()
                if nmaps == 2:
                    rows = slice(m * 64, (m + 1) * 64)
                else:
                    rows = slice(0, krows)
                mm(p[:, :], KT[rows, ks], QT[rows, qs], True, True, [KT, QT], [p])
                pt = PTs.next()
                act(pt[:, :], p[:, :], AF.Exp, [p], [pt], scale=scale)
                if nmaps == 2:
                    dve(lambda: nc.vector.tensor_tensor(pt[:, :], pt[:, :], et[:, :], ALU.mult), [pt, et], [pt])
                PTl.append(pt)
            return PTl

        def pv(kt, PTl):
            for m in range(nmaps):
                mm(accO[m][0:dv, :], Vv[:, kt, :], PTl[m][:, :], kt == 0, kt == 15, [Vt, PTl[m]], [accO[m]])
                mm(accS[m][0:dv, :], ones_bf[:, 0:dv], PTl[m][:, :], kt == 0, kt == 15, [ones_bf, PTl[m]], [accS[m]])

        for qb in range(4):
            cur = scores(qb, 0)
            for kt in range(16):
                nxt = scores(qb, kt + 1) if kt < 15 else None
                pv(kt, cur)
                cur = nxt
            finish_fn(qb, accO, accS)

    def run_seq(s):
        nonlocal arena_prev
        for c in range(8):
            for tb in range(4):
                E.dma("sp", xT[c][tb][:, :], xT_d[s, c * 128:(c + 1) * 128, tb * 512:(tb + 1) * 512],
                      reads=[Din], writes=[xT[c][tb]])
        rd = inherit(arena_prev)
        posb_i = T(arena[:, 0:4096].bitcast(I32), "posb_i", rd)
        posb = T(arena[:, 4096:8192].bitcast(F32), "posb", rd)
        ang = T(arena[:, 8192:12288].bitcast(F32), "ang", rd)
        ang2 = T(arena[:, 12288:16384].bitcast(F32), "ang2", rd)
        etmp = Rot([T(arena[:, 16384 + i * 2048:16384 + (i + 1) * 2048], "etmp%d" % i, rd) for i in range(2)])
        E.dma("sp", posb_i[:, :], posb_d[s], reads=[Din], writes=[posb_i])
        E.dma("sp", posk_i[:, :], posk_d[s], reads=[Din], writes=[posk_i])
        dve(lambda: nc.vector.tensor_copy(posb[:, :], posb_i[:, :]), [posb_i], [posb])
        dve(lambda: nc.vector.tensor_copy(posk[:, :], posk_i[:, :]), [posk_i], [posk])
        angi = T(arena[:, 20480:20480 + 2048].bitcast(I32), "angi", rd)

        def reduce_angle(dst, src, shift):
            for hf in range(2):
                cs = slice(hf * 1024, (hf + 1) * 1024)
                dve(lambda: nc.vector.tensor_scalar(dst[:, cs], src[:, cs], shift, 1.0 / TWO_PI, ALU.add, ALU.mult), [src], [dst])
                dve(lambda: nc.vector.tensor_copy(angi[:, :], dst[:, cs]), [dst], [angi])
                dve(lambda: nc.vector.tensor_copy(dst[:, cs], angi[:, :]), [angi], [dst])
                dve(lambda: nc.vector.scalar_tensor_tensor(dst[:, cs], dst[:, cs], -TWO_PI, src[:, cs], ALU.mult, ALU.add), [dst, src], [dst])
                dve(lambda: nc.vector.tensor_scalar(dst[:, cs], dst[:, cs], -3.1415925 - shift, 3.1415925 - shift, ALU.max, ALU.min), [dst], [dst])
                dve(lambda: nc.vector.tensor_scalar(dst[:, cs], dst[:, cs], shift, None, ALU.add), [dst], [dst])
        dve(lambda: nc.vector.tensor_scalar(ang[:, :], posb[:, :], vcol("freq", 0), None, ALU.mult), [posb, vecs], [ang])
        reduce_angle(ang2, ang, math.pi / 2)
        act(tabC[:, :], ang2[:, :], AF.Sin, [ang2], [tabC])
        reduce_angle(ang2, ang, 0.0)
        act(ang[:, :], ang2[:, :], AF.Sin, [ang2], [ang])
        dve(lambda: nc.vector.tensor_scalar(tabS[:, :], ang[:, :], vcol("nsgn", 0), None, ALU.mult), [ang, vecs], [tabS])
        dve(lambda: nc.vector.tensor_scalar(posk[:, :], posk[:, :], -1.0, None, ALU.mult), [posk], [posk])
        for kt in range(16):
            act(ang[:, :], posb[:, :], AF.Abs, [posb, posk], [ang], bias=posk[:, kt:kt + 1], scale=1.0)
            for h in range(8):
                slope = 2.0 ** (-(h + 1))
                et = etmp.next()
                act(et[:, :], ang[:, :], AF.Exp, [ang], [et], scale=-slope)
                E.dma("sp", etab_d[s, h, kt * 128:(kt + 1) * 128, :], et[:, :], reads=[et], writes=[Tetab[s][h]])
        arena_prev = [posb_i, posb, ang, ang2, angi] + etmp.items

        for l in range(n_layers):
            lam_init = 0.8 - 0.6 * math.exp(-0.3 * l)
            if stop == 'tables':
                raise Stop()
            if l == 0:
                for c in range(8):
                    for tb in range(4):
                        dve(lambda: nc.vector.tensor_scalar(hT[c][tb][:, :], xT[c][tb][:, :], dvcol(l, s, A1, c),
                                                            modcol(l, 0, s, c), ALU.mult, ALU.add),
                            [xT[c][tb], dvt, mod], [hT[c][tb]])

            if stop == 'p1':
                raise Stop()
            rd = inherit(arena_prev)
            latn = [[T(arena[:, (j * 4 + tb) * 512:(j * 4 + tb + 1) * 512], "latn", rd) for tb in range(4)] for j in range(5)]
            o0 = 5 * 2048
            krT = T(arena[:, o0:o0 + S], "krT", rd)
            o0 += S
            QTm = Rot([T(arena[:, o0 + i * S:o0 + (i + 1) * S], "QTm%d" % i, rd) for i in range(2)])
            o0 += 2 * S
            KTm = Rot([T(arena[:, o0 + i * S:o0 + (i + 1) * S], "KTm%d" % i, rd) for i in range(2)])
            o0 += 2 * S
            Vm = Rot([T(arena[:, o0 + i * 1040:o0 + (i + 1) * 1040], "Vm%d" % i, rd) for i in range(2)])
            o0 += 2 * 1040
            assert o0 <= ARENA_N

            wl = [wload(w_lat_d[l, j], 1024) for j in range(5)]
            for tb in range(4):
                for grp, (j0, j1, n, gname) in enumerate(((0, 3, 384, "qg"), (3, 5, 256, "kvg"))):
                    pq = pb[6]
                    for j in range(j0, j1):
                        p = gp.next()
                        for kc in range(8):
                            mm(p[:, :], wl[j][:, kc * 128:(kc + 1) * 128], hT[kc][tb][:, :], kc == 0, kc == 7,
                               [wl[j], hT[kc][tb]], [p])
                        sq = tmpB.next()
                        dve(lambda: nc.vector.tensor_copy(latn[j][tb][:, :], p[:, :]), [p], [latn[j][tb]])
                        act(sq[:, :], latn[j][tb][:, :], AF.Square, [latn[j][tb]], [sq])
                        mm(pq[:, :], ones_bf[:, :], sq[:, :], j == j0, j == j1 - 1, [ones_bf, sq], [pq])
                    sd = tmpA.next()
                    act(sd[:, :], pq[:, :], AF.Sqrt, [pq], [sd], bias=RMS_EPS, scale=1.0 / n)
                    dve(lambda: nc.vector.reciprocal(sd[:, :], sd[:, :]), [sd], [sd])
                    for j in range(j0, j1):
                        gi = (l * 3 + j) if grp == 0 else (l * 2 + j - 3)
                        dve(lambda: nc.vector.scalar_tensor_tensor(latn[j][tb][:, :], latn[j][tb][:, :], vcol(gname, gi),
                                                                   sd[:, :], ALU.mult, ALU.mult),
                            [latn[j][tb], vecs, sd], [latn[j][tb]])

            if stop == 'lat':
                raise Stop()
            wka = wload(w_kr_d[l, 0], 768)
            wkb = wload(w_kr_d[l, 1], 768)
            for tb in range(4):
                ts_ = slice(tb * 512, (tb + 1) * 512)
                pa = gp.next()
                pb_ = gp.next()
                for kc in range(8):
                    mm(pa[0:96, :], wka[:, kc * 96:(kc + 1) * 96], hT[kc][tb][:, :], kc == 0, kc == 7, [wka, hT[kc][tb]], [pa])
                for kc in range(8):
                    mm(pb_[0:96, :], wkb[:, kc * 96:(kc + 1) * 96], hT[kc][tb][:, :], kc == 0, kc == 7, [wkb, hT[kc][tb]], [pb_])
                t1 = tmpA.next()
                t2 = tmpA.next()
                dve(lambda: nc.vector.tensor_tensor(t1[0:96, :], pa[0:96, :], tabC[0:96, ts_], ALU.mult), [pa, tabC], [t1])
                dve(lambda: nc.vector.tensor_tensor(t2[0:96, :], pb_[0:96, :], tabS[0:96, ts_], ALU.mult), [pb_, tabS], [t2])
                dve(lambda: nc.vector.tensor_tensor(krT[0:96, ts_], t1[0:96, :], t2[0:96, :], ALU.add), [t1, t2], [krT])

            if stop == 'kr':
                raise Stop()
            for h in range(8):
                wqa = wload(w_q_d[l, h, 0], 288)
                wqb = wload(w_q_d[l, h, 1], 288)
                wkn = wload(w_kn_d[l, h], 128)
                wv = wload(w_v_d[l, h], 128)
                QT = QTm.next()
                KT = KTm.next()
                Vt = Vm.next()
                for tb in range(4):
                    ts_ = slice(tb * 512, (tb + 1) * 512)
                    pa = gp.next()
                    pb_ = gp.next()
                    for kc in range(3):
                        mm(pa[0:96, :], wqa[:, kc * 96:(kc + 1) * 96], latn[kc][tb][:, :], kc == 0, kc == 2, [wqa, latn[kc][tb]], [pa])
                    for kc in range(3):
                        mm(pb_[0:96, :], wqb[:, kc * 96:(kc + 1) * 96], latn[kc][tb][:, :], kc == 0, kc == 2, [wqb, latn[kc][tb]], [pb_])
                    t1 = tmpA.next()
                    t2 = tmpA.next()
                    dve(lambda: nc.vector.tensor_tensor(t1[0:96, :], pa[0:96, :], tabC[0:96, ts_], ALU.mult), [pa, tabC], [t1])
                    dve(lambda: nc.vector.tensor_tensor(t2[0:96, :], pb_[0:96, :], tabS[0:96, ts_], ALU.mult), [pb_, tabS], [t2])
                    dve(lambda: nc.vector.tensor_tensor(QT[0:96, ts_], t1[0:96, :], t2[0:96, :], ALU.add), [t1, t2], [QT])
                    pk = gp.next()
                    for kc in range(2):
                        mm(pk[0:64, :], wkn[:, kc * 64:(kc + 1) * 64], latn[3 + kc][tb][:, :], kc == 0, kc == 1,
                           [wkn, latn[3 + kc][tb]], [pk])
                    act(KT[0:64, ts_], pk[0:64, :], AF.Copy, [pk], [KT])
                    dve(lambda: nc.vector.tensor_copy(KT[64:96, ts_], krT[64:96, ts_]), [krT], [KT])
                Vv = Vt[:, 0:1024].rearrange("p (t d) -> p t d", d=64)
                for tg in range(4):
                    pv = gp.next()
                    for ti in range(4):
                        tt = tg * 4 + ti
                        tb, off = tt // 4, (tt % 4) * 128
                        for kc in range(2):
                            mm(pv[:, ti * 64:(ti + 1) * 64], latn[3 + kc][tb][:, off:off + 128], wv[:, kc * 64:(kc + 1) * 64],
                               (kc == 0 and ti == 0), kc == 1, [latn[3 + kc][tb], wv], [pv], skip_group_check=True)
                    dve(lambda: nc.vector.tensor_copy(Vv[:, tg * 4:(tg + 1) * 4, :],
                                                      pv[:, 0:256].rearrange("p (t d) -> p t d", d=64)), [pv], [Vt])

                def fin_mla(qb, accO, accS, h=h):
                    r = tmpA.next()
                    dve(lambda: nc.vector.reciprocal(r[0:64, :], accS[0][0:64, :]), [accS[0]], [r])
                    st = OTst.next()
                    dve(lambda: nc.vector.tensor_tensor(st[0:64, :], accO[0][0:64, :], r[0:64, :], ALU.mult), [accO[0], r], [st])
                    E.dma("sp", otm_d[h * 64:(h + 1) * 64, qb * 512:(qb + 1) * 512], st[0:64, :], reads=[st], writes=[Totm[qb]])

                attention(QT, KT, Vt, 96, MLA_SCALE, 1, 64, s, h, fin_mla)

            if stop == 'mla':
                raise Stop()
            rd = inherit([t for row in latn for t in row] + [krT] + QTm.items + KTm.items + Vm.items)
            o0 = 0
            QTd = Rot([T(arena[:, o0 + i * S:o0 + (i + 1) * S], "QTd%d" % i, rd) for i in range(2)])
            o0 += 2 * S
            KTd = Rot([T(arena[:, o0 + i * S:o0 + (i + 1) * S], "KTd%d" % i, rd) for i in range(2)])
            o0 += 2 * S
            Vd = Rot([T(arena[:, o0 + i * 2064:o0 + (i + 1) * 2064], "Vd%d" % i, rd) for i in range(2)])
            o0 += 2 * 2064
            assert o0 <= ARENA_N
            for h in range(8):
                wdq = wload(w_dq_d[l, h], 1024)
                wdk = wload(w_dk_d[l, h], 1024)
                wdv = wload(w_dv_d[l, h], 1024)
                QT = QTd.next()
                KT = KTd.next()
                Vt = Vd.next()
                for tb in range(4):
                    ts_ = slice(tb * 512, (tb + 1) * 512)
                    for (w_, dst, eng_) in ((wdq, QT, "act"), (wdk, KT, "dve")):
                        p = gp.next()
                        for kc in range(8):
                            mm(p[:, :], w_[:, kc * 128:(kc + 1) * 128], hT[kc][tb][:, :], kc == 0, kc == 7, [w_, hT[kc][tb]], [p])
                        if eng_ == "act":
                            act(dst[:, ts_], p[:, :], AF.Copy, [p], [dst])
                        else:
                            dve(lambda: nc.vector.tensor_copy(dst[:, ts_], p[:, :]), [p], [dst])
                Vv = Vt[:, 0:2048].rearrange("p (t d) -> p t d", d=128)
                for tg in range(4):
                    pv = gp.next()
                    for ti in range(4):
                        tt = tg * 4 + ti
                        tb, off = tt // 4, (tt % 4) * 128
                        for kc in range(8):
                            mm(pv[:, ti * 128:(ti + 1) * 128], hT[kc][tb][:, off:off + 128], wdv[:, kc * 128:(kc + 1) * 128],
                               (kc == 0 and ti == 0), kc == 7, [hT[kc][tb], wdv], [pv], skip_group_check=True)
                    act(Vv[:, tg * 4:(tg + 1) * 4, :], pv[:, :].rearrange("p (t d) -> p t d", d=128), AF.Copy, [pv], [Vt])

                def fin_diff(qb, accO, accS, h=h, l=l, lam_init=lam_init):
                    r0, r1, o0 = tmpA.next(), tmpA.next(), tmpA.next()
                    dve(lambda: nc.vector.reciprocal(r0[:, :], accS[0][:, :]), [accS[0]], [r0])
                    dve(lambda: nc.vector.reciprocal(r1[:, :], accS[1][:, :]), [accS[1]], [r1])
                    dve(lambda: nc.vector.tensor_tensor(o0[:, :], accO[0][:, :], r0[:, :], ALU.mult), [accO[0], r0], [o0])
                    dve(lambda: nc.vector.scalar_tensor_tensor(r1[:, :], accO[1][:, :], nlam[:, l:l + 1], r1[:, :], ALU.mult, ALU.mult),
                        [accO[1], nlam, r1], [r1])
                    dve(lambda: nc.vector.tensor_tensor(o0[:, :], o0[:, :], r1[:, :], ALU.add), [o0, r1], [o0])
                    sq = tmpB.next()
                    act(sq[:, :], o0[:, :], AF.Square, [o0], [sq])
                    pr = gps.next()
                    mm(pr[:, :], ones_bf[:, :], sq[:, :], True, True, [ones_bf, sq], [pr])
                    k2 = (1.0 - lam_init) ** 2
                    act(r0[:, :], pr[:, :], AF.Sqrt, [pr], [r0], bias=RMS_EPS / k2, scale=1.0 / (128.0 * k2))
                    dve(lambda: nc.vector.reciprocal(r0[:, :], r0[:, :]), [r0], [r0])
                    st = OTst.next()
                    dve(lambda: nc.vector.scalar_tensor_tensor(st[:, :], o0[:, :], vcol("dng", l * 8 + h), r0[:, :], ALU.mult, ALU.mult),
                        [o0, vecs, r0], [st])
                    E.dma("sp", otd_d[h * 128:(h + 1) * 128, qb * 512:(qb + 1) * 512], st[:, :], reads=[st], writes=[Totd[qb]])

                attention(QT, KT, Vt, 64, DIFF_SCALE, 2, 128, s, h, fin_diff)

            if stop == 'diff':
                raise Stop()
            rd = inherit(QTd.items + KTd.items + Vd.items)
            otmT = T(arena[:, 0:2048], "otmT", rd)
            otdT = T(arena[:, 2048:6144], "otdT", rd)
            mrg = [T(arena[:, 6144 + c * 512:6144 + (c + 1) * 512], "mrg%d" % c, rd) for c in range(8)]
            is_moe = (l % 2 == 1)
            for tb in range(4):
                ts_ = slice(tb * 512, (tb + 1) * 512)
                for kc in range(4):
                    E.dma("sp", otmT[:, kc * 512:(kc + 1) * 512], otm_d[kc * 128:(kc + 1) * 128, ts_], reads=[Totm[tb]], writes=[otmT])
                for kc in range(8):
                    E.dma("sp", otdT[:, kc * 512:(kc + 1) * 512], otd_d[kc * 128:(kc + 1) * 128, ts_], reads=[Totd[tb]], writes=[otdT])
                for dc in range(8):
                    wbm = wload(w_brm_d[l, dc], 512)
                    wbd = wload(w_brd_d[l, dc], 1024)
                    wga = wload(w_gt_d[l, dc], 1024)
                    wgb = wload(w_gt_d[l, 8 + dc], 1024)
                    pya, pyb, pga, pgb = gp.next(), gp.next(), gp.next(), gp.next()
                    for kc in range(4):
                        mm(pya[:, :], wbm[:, kc * 128:(kc + 1) * 128], otmT[:, kc * 512:(kc + 1) * 512], kc == 0, kc == 3, [wbm, otmT], [pya])
                    for kc in range(8):
                        mm(pyb[:, :], wbd[:, kc * 128:(kc + 1) * 128], otdT[:, kc * 512:(kc + 1) * 512], kc == 0, kc == 7, [wbd, otdT], [pyb])
                    for kc in range(8):
                        mm(pga[:, :], wga[:, kc * 128:(kc + 1) * 128], hT[kc][tb][:, :], kc == 0, kc == 7, [wga, hT[kc][tb]], [pga])
                    for kc in range(8):
                        mm(pgb[:, :], wgb[:, kc * 128:(kc + 1) * 128], hT[kc][tb][:, :], kc == 0, kc == 7, [wgb, hT[kc][tb]], [pgb])
                    sa, sb_ = tmpA.next(), tmpA.next()
                    act(sa[:, :], pga[:, :], AF.Sigmoid, [pga], [sa])
                    act(sb_[:, :], pgb[:, :], AF.Sigmoid, [pgb], [sb_])
                    dve(lambda: nc.vector.tensor_tensor(sa[:, :], sa[:, :], pya[:, :], ALU.mult), [sa, pya], [sa])
                    dve(lambda: nc.vector.tensor_tensor(sb_[:, :], sb_[:, :], pyb[:, :], ALU.mult), [sb_, pyb], [sb_])
                    dve(lambda: nc.vector.tensor_tensor(mrg[dc][:, :], sa[:, :], sb_[:, :], ALU.add), [sa, sb_], [mrg[dc]])
                for dc in range(8):
                    wo = wload(w_out_d[l, dc], 1024)
                    p = gp.next()
                    for kc in range(8):
                        mm(p[:, :], wo[:, kc * 128:(kc + 1) * 128], mrg[kc][:, :], kc == 0, kc == 7, [wo, mrg[kc]], [p])
                    x_ = xT[dc][tb]
                    dve(lambda: nc.vector.scalar_tensor_tensor(x_[:, :], p[:, :], dvcol(l, s, G1, dc), x_[:, :], ALU.mult, ALU.add),
                        [p, dvt, x_], [x_])
                layernorm(tb, "ln1_g", "ln1_b", l, GA2, GB2, s, True)

            if stop == 'merge':
                raise Stop()
            rd = inherit([otmT, otdT] + mrg)
            gTt = T(arena[:, 0:ARENA_N], "gT", rd)
            if is_moe:
                li = l // 2
            nexp = NEXP if is_moe else 1
            for tbb in range(2):
                if is_moe:
                    for t8 in range(8):
                        tb, off = (tbb * 8 + t8) // 4, ((tbb * 8 + t8) % 4) * 128
                        p = gp.next()
                        for kc in range(8):
                            mm(p[:, 0:8], hT[kc][tb][:, off:off + 128], rwb[li][:, kc * 8:(kc + 1) * 8], kc == 0, kc == 7, [hT[kc][tb], rwb[li]], [p])
                        lg = lgt.next()
                        rbv = vecs[:, V("rb") + li * 8: V("rb") + li * 8 + 8]
                        dve(lambda: nc.vector.tensor_tensor(lg[:, 0:8], p[:, 0:8], rbv, ALU.add), [p, vecs], [lg])
                        dve(lambda: nc.vector.reduce_max(lg[:, 32:33], lg[:, 0:8], axis=AX.X), [lg], [lg])
                        dve(lambda: nc.vector.tensor_scalar(lg[:, 8:16], lg[:, 0:8], lg[:, 32:33], None, ALU.is_equal), [lg], [lg])
                        dve(lambda: nc.vector.scalar_tensor_tensor(lg[:, 16:24], lg[:, 8:16], -1e30, lg[:, 0:8], ALU.mult, ALU.add), [lg], [lg])
                        dve(lambda: nc.vector.reduce_max(lg[:, 33:34], lg[:, 16:24], axis=AX.X), [lg], [lg])
                        dve(lambda: nc.vector.tensor_scalar(lg[:, 24:32], lg[:, 16:24], lg[:, 33:34], None, ALU.is_equal), [lg], [lg])
                        dve(lambda: nc.vector.tensor_tensor(lg[:, 34:35], lg[:, 32:33], lg[:, 33:34], ALU.subtract), [lg], [lg])
                        act(lg[:, 35:36], lg[:, 34:35], AF.Sigmoid, [lg], [lg])
                        act(lg[:, 36:37], lg[:, 34:35], AF.Sigmoid, [lg], [lg], scale=-1.0)
                        dve(lambda: nc.vector.tensor_scalar(lg[:, 8:16], lg[:, 8:16], lg[:, 35:36], None, ALU.mult), [lg], [lg])
                        dve(lambda: nc.vector.scalar_tensor_tensor(lg[:, 40:48], lg[:, 24:32], lg[:, 36:37], lg[:, 8:16], ALU.mult, ALU.add), [lg], [lg])
                        dve(lambda: nc.vector.tensor_copy(lgb[:, 0:8], lg[:, 40:48]), [lg], [lgb])
                        E.op("pe", lambda: nc.tensor.transpose(ptb[0:8, 0:128], lgb[:, 0:8], ident_bf[:, :]), [lgb, ident_bf], [ptb])
                        dve(lambda: nc.vector.tensor_copy(GT[0:8, t8 * 128:(t8 + 1) * 128], ptb[0:8, 0:128]), [ptb], [GT])
                for e in range(nexp):
                    ei = eidx(l, e)
                    if is_moe:
                        g_ = gb.next()
                        for half in range(2):
                            p = gp.next()
                            mm(p[:, :], sel[0:8, e * 128:(e + 1) * 128], GT[0:8, half * 512:(half + 1) * 512], True, True, [sel, GT], [p])
                            act(g_[:, half * 512:(half + 1) * 512], p[:, :], AF.Copy, [p], [g_])
                    for fc in range(NFC):
                        w1 = wload(w1_d[ei, fc], 1024)
                        w3 = wload(w3_d[ei, fc], 1024)
                        for half in range(2):
                            tb = tbb * 2 + half
                            pa = gp.next()
                            pb_ = gp.next()
                            for kc in range(8):
                                mm(pa[:, :], w1[:, kc * 128:(kc + 1) * 128], hT[kc][tb][:, :], kc == 0, kc == 7, [w1, hT[kc][tb]], [pa])
                            for kc in range(8):
                                mm(pb_[:, :], w3[:, kc * 128:(kc + 1) * 128], hT[kc][tb][:, :], kc == 0, kc == 7, [w3, hT[kc][tb]], [pb_])
                            sa = tmpB.next()
                            act(sa[:, :], pa[:, :], AF.Silu, [pa], [sa])
                            gsl = gTt[:, fc * 1024 + half * 512: fc * 1024 + (half + 1) * 512]
                            if is_moe:
                                sa2 = tmpB.next()
                                dve(lambda: nc.vector.tensor_tensor(sa2[:, :], sa[:, :], pb_[:, :], ALU.mult), [sa, pb_], [sa2])
                                dve(lambda: nc.vector.tensor_tensor(gsl, sa2[:, :], g_[:, half * 512:(half + 1) * 512], ALU.mult), [sa2, g_], [gTt])
                            else:
                                dve(lambda: nc.vector.tensor_tensor(gsl, sa[:, :], pb_[:, :], ALU.mult), [sa, pb_], [gTt])
                    for dc in range(8):
                        w2h = [wload(w2_d[ei, dc, 0], 1024), wload(w2_d[ei, dc, 1], 1024), wload(w2_d[ei, dc, 2][:, 0:768], 768)]
                        for half in range(2):
                            tb = tbb * 2 + half
                            p = gp.next()
                            for fc in range(NFC):
                                w2 = w2h[fc // 8]
                                mm(p[:, :], w2[:, (fc % 8) * 128:(fc % 8 + 1) * 128], gTt[:, fc * 1024 + half * 512: fc * 1024 + (half + 1) * 512],
                                   fc == 0, fc == NFC - 1, [w2, gTt], [p])
                            x_ = xT[dc][tb]
                            dve(lambda: nc.vector.scalar_tensor_tensor(x_[:, :], p[:, :], dvcol(l, s, G2, dc), x_[:, :], ALU.mult, ALU.add),
                                [p, dvt, x_], [x_])
                for half in range(2):
                    layernorm(tbb * 2 + half, "ln2_g", "ln2_b", l, GAN, GBN, s, l < n_layers - 1)
            arena_prev = [gTt]


    for s in range(n_seq):
        try:
            run_seq(s)
        except Stop:
            pass
        for c in range(8):
            for tb in range(4):
                E.dma("sp", outT_d[s, c * 128:(c + 1) * 128, tb * 512:(tb + 1) * 512], xT[c][tb][:, :],
                      reads=[xT[c][tb]], writes=[Tout[s]])
    E.finish(Tout)
    return nc, em


def tile_w(W, B):
    K, N = W.shape
    KC = K // 128
    NB = N // B
    return np.ascontiguousarray(W.reshape(KC, 128, NB, B).transpose(2, 1, 0, 3)).reshape(NB, 128, KC * B)


def prep_shared(inp):
    f = lambda a: np.asarray(a, dtype=np.float32)
    w_in = f(inp["w_in"])
    o = {}
    o["w_ada"] = np.stack([tile_w(f(inp["w_ada"][l]), 128) for l in range(L)])
    o["w_lat"] = np.stack([tile_w(w_in[l][:, 0:640], 128) for l in range(L)])
    z64 = np.zeros((D, 64), np.float32)
    kr = []
    for l in range(L):
        r = w_in[l][:, 640:672]
        A = np.concatenate([z64, r], axis=1)
        B = np.concatenate([z64, r[:, 16:32], r[:, 0:16]], axis=1)
        kr.append(np.stack([tile_w(A, 96)[0], tile_w(B, 96)[0]]))
    o["w_kr"] = np.stack(kr)
    c0 = 672
    o["w_dq"] = np.stack([tile_w(w_in[l][:, c0:c0 + 1024], 128) for l in range(L)])
    o["w_dk"] = np.stack([tile_w(w_in[l][:, c0 + 1024:c0 + 2048], 128) for l in range(L)])
    o["w_dv"] = np.stack([tile_w(w_in[l][:, c0 + 2048:c0 + 3072], 128) for l in range(L)])
    o["w_gt"] = np.stack([tile_w(w_in[l][:, c0 + 3072:c0 + 5120], 128) for l in range(L)])
    wq = f(inp["w_q_up"])
    qs = []
    for l in range(L):
        hs = []
        for h in range(8):
            Wh = wq[l][:, h * 96:(h + 1) * 96]
            A = Wh
            B = np.concatenate([np.zeros((384, 64), np.float32), Wh[:, 80:96], Wh[:, 64:80]], axis=1)
            hs.append(np.stack([tile_w(A, 96)[0], tile_w(B, 96)[0]]))
        qs.append(np.stack(hs))
    o["w_q"] = np.stack(qs)
    wkv = f(inp["w_kv_up"])
    o["w_kn"] = np.stack([np.stack([tile_w(wkv[l][:, h * 128:h * 128 + 64], 64)[0] for h in range(8)]) for l in range(L)])
    o["w_v"] = np.stack([np.stack([tile_w(wkv[l][:, h * 128 + 64:h * 128 + 128], 64)[0] for h in range(8)]) for l in range(L)])
    o["w_brm"] = np.stack([tile_w(f(inp["w_br_mla"][l]), 128) for l in range(L)])
    o["w_brd"] = np.stack([tile_w(f(inp["w_br_diff"][l]), 128) for l in range(L)])
    o["w_out"] = np.stack([tile_w(f(inp["w_out"][l]), 128) for l in range(L)])
    w1l, w3l, w2l = [], [], []
    fw1, fw3, fw2 = f(inp["ffn_w1"]), f(inp["ffn_w3"]), f(inp["ffn_w2"])
    mw1, mw3, mw2 = f(inp["moe_w1"]), f(inp["moe_w3"]), f(inp["moe_w2"])
    for i in range(2):
        w1l.append(tile_w(fw1[i], 128)); w3l.append(tile_w(fw3[i], 128)); w2l.append(tile_w(fw2[i], 128))
    for i in range(2):
        for e in range(NEXP):
            w1l.append(tile_w(mw1[i, e], 128)); w3l.append(tile_w(mw3[i, e], 128)); w2l.append(tile_w(mw2[i, e], 128))
    o["w1"] = np.stack(w1l)
    o["w3"] = np.stack(w3l)
    w2a = np.stack(w2l)
    w2p = np.zeros((NE, 8, 128, 24 * 128), np.float32)
    w2p[..., :NFC * 128] = w2a
    o["w2"] = np.ascontiguousarray(w2p.reshape(NE, 8, 128, 3, 1024).transpose(0, 1, 3, 2, 4))
    o["ident"] = np.eye(128, dtype=np.float32)
    o["rw"] = np.stack([tile_w(f(inp["router_w"][i]), 8)[0] for i in range(2)])
    vec = np.zeros((128, NV), np.float32)

    def put(name, arr):
        arr = np.asarray(arr, np.float32)
        vec[:, VL[name]:VL[name] + arr.shape[1]] = arr
    put("b_ada", f(inp["b_ada"]).reshape(L, 48, 128).transpose(2, 0, 1).reshape(128, L * 48))
    for nm, key in (("ln1_g", "ln1_g"), ("ln1_b", "ln1_b"), ("ln2_g", "ln2_g"), ("ln2_b", "ln2_b")):
        put(nm, f(inp[key]).reshape(L, 8, 128).transpose(2, 0, 1).reshape(128, L * 8))
    put("qg", f(inp["q_norm_g"]).reshape(L, 3, 128).transpose(2, 0, 1).reshape(128, L * 3))
    put("kvg", f(inp["kv_norm_g"]).reshape(L, 2, 128).transpose(2, 0, 1).reshape(128, L * 2))
    put("dng", f(inp["diff_norm_g"]).reshape(L, 8, 128).transpose(2, 0, 1).reshape(128, L * 8))
    lam = np.stack([f(inp["lambda_q1"]), f(inp["lambda_k1"]), f(inp["lambda_q2"]), f(inp["lambda_k2"])], axis=1)
    o["lam"] = np.ascontiguousarray(np.broadcast_to(lam.reshape(1, L * 256), (128, L * 256)))
    put("rb", np.broadcast_to(f(inp["router_b"]).reshape(1, 16), (128, 16)))
    inv_freq = (np.float32(10000.0) ** (-np.arange(0, 32, 2, dtype=np.float32) / np.float32(32))).astype(np.float32)
    fr = np.zeros((128, 1), np.float32)
    fr[64:80, 0] = inv_freq
    fr[80:96, 0] = inv_freq
    put("freq", fr)
    ns = np.zeros((128, 1), np.float32)
    ns[64:80, 0] = -1.0
    ns[80:96, 0] = 1.0
    put("nsgn", ns)
    o["vecs"] = vec
    sel = np.zeros((8, 8, 128), np.float32)
    for e in range(8):
        sel[e, e, :] = 1.0
    o["sel"] = sel.reshape(8, 1024)
    return o


def prep_core(inp, core):
    x = np.asarray(inp["x"], np.float32)[core * NS:(core + 1) * NS]
    c = np.asarray(inp["c"], np.float32)[core * NS:(core + 1) * NS]
    pos = np.asarray(inp["positions"], np.int32)[core * NS:(core + 1) * NS]
    o = {}
    o["xT"] = np.ascontiguousarray(x.transpose(0, 2, 1))
    o["posb"] = np.ascontiguousarray(np.broadcast_to(pos[:, None, :], (NS, 128, S)))
    o["posk"] = np.ascontiguousarray(pos.reshape(NS, 16, 128).transpose(0, 2, 1))
    o["cT"] = np.ascontiguousarray(c.reshape(NS, 8, 128).transpose(2, 1, 0)).reshape(128, 8 * NS)
    return o


_CACHE = {}


def kernel(**inputs):
    n = 8
    shared = prep_shared(inputs)
    if "nc" not in _CACHE:
        _CACHE["nc"] = build()[0]
    nc = _CACHE["nc"]
    in_maps = []
    for core in range(n):
        m = dict(shared)
        m.update(prep_core(inputs, core))
        in_maps.append(m)
    res = run_bass_kernel_spmd(nc, in_maps, core_ids=list(range(n)))
    outs = [np.asarray(r["outT"]).transpose(0, 2, 1) for r in res.results]
    return np.ascontiguousarray(np.concatenate(outs, axis=0)).astype(np.float32)
```

```python
import math
import numpy as np
from contextlib import ExitStack
import concourse.bass as bass
import concourse.mybir as mybir
from concourse.bass_utils import run_bass_kernel_spmd

F32 = mybir.dt.float32
BF16 = mybir.dt.bfloat16
I32 = mybir.dt.int32
AF = mybir.ActivationFunctionType
ALU = mybir.AluOpType
AX = mybir.AxisListType

L = 4
D = 1024
S = 2048
NS = 2
F = 2816
NFC = 22
NEXP = 8
NE = 2 + 2 * NEXP
ALPHA = (2.0 * L) ** 0.25
LN_EPS = 1e-5
RMS_EPS = 1e-6
MLA_SCALE = 96.0 ** -0.5
DIFF_SCALE = 64.0 ** -0.5
TWO_PI = 2.0 * math.pi


class T:
    __slots__ = ("ap", "name", "last_w", "readers")

    def __init__(self, ap, name="", readers=None):
        self.ap = ap
        self.name = name
        self.last_w = None
        self.readers = dict(readers) if readers else {}

    def __getitem__(self, idx):
        return self.ap[idx]


def inherit(tiles):
    r = {}
    for t in tiles:
        for k, v in t.readers.items():
            r[k] = max(r.get(k, 0), v)
        if t.last_w is not None:
            k, v = t.last_w
            r[k] = max(r.get(k, 0), v)
    return r


class Em:
    NRING = 16
    SEM_LIMIT = 30000

    def __init__(self, nc):
        self.nc = nc
        self.eng = {"pe": nc.tensor, "act": nc.scalar, "dve": nc.vector,
                    "pool": nc.gpsimd, "sp": nc.sync}
        self.sem = {}
        self.cnt = {}
        self.known = {e: {} for e in self.eng}
        self.cur = {}
        self.nep = {}
        for e in self.eng:
            self.sem[e] = nc.alloc_semaphore(name="s_" + e)
            self.cnt[e] = 0
            self.cur[e] = e
            self.nep[e] = 0
        self.ring = {}
        for q in ("sp", "pool"):
            lst = []
            for i in range(self.NRING):
                key = "r_%s%d" % (q, i)
                self.sem[key] = nc.alloc_semaphore(name=key)
                self.cnt[key] = 0
                lst.append(key)
            self.ring[q] = [lst, 0]
        self.n_ins = {e: 0 for e in self.eng}
        self.n_wait = {e: 0 for e in self.eng}

    def _need(self, e, ev):
        if ev is None:
            return
        k, v = ev
        if e == "pe" and k.split("#")[0] == "pe":
            return
        if self.known[e].get(k, 0) >= v:
            return
        self.eng[e].wait_ge(self.sem[k], v)
        self.n_wait[e] += 1
        self.known[e][k] = v

    def _deps(self, e, reads, writes):
        for t in reads:
            self._need(e, t.last_w)
        for t in writes:
            self._need(e, t.last_w)
            for k, v in list(t.readers.items()):
                self._need(e, (k, v))

    def _commit(self, ev, reads, writes):
        k, v = ev
        for t in reads:
            if t.readers.get(k, 0) < v:
                t.readers[k] = v
        for t in writes:
            t.last_w = ev
            t.readers = {}

    def op(self, e, fn, reads=(), writes=()):
        self._deps(e, reads, writes)
        ins = fn()
        if e == "pe" and len(reads) > 0 and reads[0].last_w is not None and reads[0].last_w[0].split("#")[0] != "pe":
            k0, v0 = reads[0].last_w
            ins._wait_ge(self.sem[k0], v0)
        key = self.cur[e]
        if self.cnt[key] >= self.SEM_LIMIT:
            self.nep[e] += 1
            key = "%s#%d" % (e, self.nep[e])
            self.sem[key] = self.nc.alloc_semaphore(name="s_%s_%d" % (e, self.nep[e]))
            self.cnt[key] = 0
            self.cur[e] = key
        self.cnt[key] += 1
        ins.then_inc(self.sem[key], 1)
        ev = (key, self.cnt[key])
        self._commit(ev, reads, writes)
        self.n_ins[e] += 1
        return ev

    def dma(self, q, out_ap, in_ap, reads=(), writes=(), **kw):
        self._deps(q, reads, writes)
        lst, pos = self.ring[q]
        key = lst[pos % self.NRING]
        self.ring[q][1] = pos + 1
        if self.cnt[key] > 0:
            self._need(q, (key, self.cnt[key]))
        ins = self.eng[q].dma_start(out=out_ap, in_=in_ap, **kw)
        self.cnt[key] += 16
        ins.then_inc(self.sem[key], 16)
        ev = (key, self.cnt[key])
        self._commit(ev, reads, writes)
        self.n_ins[q] += 1
        return ev

    def finish(self, tiles):
        for t in tiles:
            self._need("sp", t.last_w)
        for e in self.eng:
            key = self.cur[e]
            if e != "sp" and self.cnt[key] > 0:
                self._need("sp", (key, self.cnt[key]))


class Rot:
    def __init__(self, items):
        self.items = items
        self.i = 0

    def next(self):
        t = self.items[self.i % len(self.items)]
        self.i += 1
        return t


def vlay():
    lay = {}
    off = 0
    for name, w in (("b_ada", L * 48), ("ln1_g", L * 8), ("ln1_b", L * 8), ("ln2_g", L * 8),
                    ("ln2_b", L * 8), ("qg", L * 3), ("kvg", L * 2), ("dng", L * 8),
                    ("rb", 2 * 8), ("freq", 1), ("nsgn", 1)):
        lay[name] = off
        off += w
    return lay, off


VL, NV = vlay()


def eidx(l, e):
    return (l // 2) if l % 2 == 0 else 2 + (l // 2) * NEXP + e


class Stop(Exception):
    pass


def build(n_layers=L, n_seq=NS, stop=None):
    nc = bass.Bass("TRN2", target_bir_lowering=False)
    es = ExitStack()

    def din(name, shape, dt=F32):
        return nc.dram_tensor(name, list(shape), dt, kind="ExternalInput").ap()

    Ld = n_layers
    NEd = max(eidx(l, e) for l in range(n_layers) for e in range(NEXP if l % 2 else 1)) + 1
    xT_d = din("xT", [NS, D, S])
    posb_d = din("posb", [NS, 128, S], I32)
    posk_d = din("posk", [NS, 128, 16], I32)
    cT_d = din("cT", [128, 8 * NS])
    vecs_d = din("vecs", [128, NV])
    sel_d = din("sel", [8, 8 * 128])
    ident_d = din("ident", [128, 128])
    lam_d = din("lam", [128, L * 256])
    w_ada_d = din("w_ada", [Ld, 48, 128, 1024])
    w_lat_d = din("w_lat", [Ld, 5, 128, 1024])
    w_kr_d = din("w_kr", [Ld, 2, 128, 8 * 96])
    w_dq_d = din("w_dq", [Ld, 8, 128, 1024])
    w_dk_d = din("w_dk", [Ld, 8, 128, 1024])
    w_dv_d = din("w_dv", [Ld, 8, 128, 1024])
    w_gt_d = din("w_gt", [Ld, 16, 128, 1024])
    w_q_d = din("w_q", [Ld, 8, 2, 128, 3 * 96])
    w_kn_d = din("w_kn", [Ld, 8, 128, 128])
    w_v_d = din("w_v", [Ld, 8, 128, 128])
    w_brm_d = din("w_brm", [Ld, 8, 128, 512])
    w_brd_d = din("w_brd", [Ld, 8, 128, 1024])
    w_out_d = din("w_out", [Ld, 8, 128, 1024])
    w1_d = din("w1", [NEd, NFC, 128, 1024])
    w3_d = din("w3", [NEd, NFC, 128, 1024])
    w2_d = din("w2", [NEd, 8, 3, 128, 1024])
    rw_d = din("rw", [2, 128, 64])
    outT_d = nc.dram_tensor("outT", [NS, D, S], F32, kind="ExternalOutput").ap()
    etab_d = nc.dram_tensor("etab", [NS, 8, S, S], BF16).ap()
    otm_d = nc.dram_tensor("otm", [512, S], BF16).ap()
    otd_d = nc.dram_tensor("otd", [1024, S], BF16).ap()

    em = Em(nc)
    E = em

    def sbt(name, shape, dt):
        return es.enter_context(nc.sbuf_tensor("sb_" + name, list(shape), dt))

    def sb(name, shape, dt):
        return T(sbt(name, shape, dt), name)

    def pst(name, shape, dt):
        return es.enter_context(nc.psum_tensor("ps_" + name, list(shape), dt))

    Din = T(None, "dram_in")
    Tout = [T(None, "out%d" % s) for s in range(NS)]
    Tetab = [[T(None, "etab") for h in range(8)] for s in range(NS)]
    Totm = [T(None, "otm%d" % tb) for tb in range(4)]
    Totd = [T(None, "otd%d" % tb) for tb in range(4)]

    xT_t = sbt("xT_t", [128, 8 * S], F32)
    hT_t = sbt("hT_t", [128, 8 * S], BF16)
    xT = [[T(xT_t[:, c * S + tb * 512: c * S + (tb + 1) * 512], "x%d_%d" % (c, tb)) for tb in range(4)]
          for c in range(8)]
    hT = [[T(hT_t[:, c * S + tb * 512: c * S + (tb + 1) * 512], "h%d_%d" % (c, tb)) for tb in range(4)]
          for c in range(8)]
    vecs = sb("vecs", [128, NV], F32)
    cT = sb("cT", [128, 8 * NS], F32)
    cond = sb("cond", [128, 8 * NS], F32)
    mod = sb("mod", [128, L * 48 * NS], F32)
    NDV = 8
    dvt = sb("dvt", [128, L * NS * NDV * 8], F32)
    nlam = sb("nlam", [128, L], F32)
    lamtmp = sb("lamtmp", [128, 64 + 4], F32)
    ones_bf = sb("ones_bf", [128, 128], BF16)
    ones_f = sb("ones_f", [128, 128], F32)
    ident_bf = sb("ident_bf", [128, 128], BF16)
    ident_f = sb("ident_f", [128, 128], F32)
    sel = sb("sel", [8, 8 * 128], BF16)
    rw = [sb("rw%d" % i, [128, 64], F32) for i in range(2)]
    rwp = sb("rwp", [128, 64], F32)
    brow = sb("brow", [1, 8], F32)
    tabC = sb("tabC", [128, S], BF16)
    tabS = sb("tabS", [128, S], BF16)
    posk_i = sb("posk_i", [128, 16], I32)
    posk = sb("posk", [128, 16], F32)
    NWS = 11
    wslots = Rot([sb("wsl%d" % i, [128, 1024], BF16) for i in range(NWS)])
    PTs = Rot([sb("PT%d" % i, [128, 512], BF16) for i in range(6)])
    Ets = Rot([sb("Et%d" % i, [128, 512], BF16) for i in range(3)])
    OTst = Rot([sb("OTst%d" % i, [128, 512], BF16) for i in range(2)])
    tmpA = Rot([sb("tmpA%d" % i, [128, 512], F32) for i in range(3)])
    tmpB = Rot([sb("tmpB%d" % i, [128, 512], BF16) for i in range(3)])
    small = Rot([sb("small%d" % i, [128, 16], F32) for i in range(6)])
    lgt = Rot([sb("lgt%d" % i, [128, 64], F32) for i in range(2)])
    GT = sb("GT", [8, 1024], BF16)
    gb = Rot([sb("gb%d" % i, [128, 1024], BF16) for i in range(1)])
    ARENA_N = 22560
    arena = sbt("arena", [128, ARENA_N], BF16)

    pb = [T(pst("pb%d" % i, [128, 512], F32), "pb%d" % i) for i in range(7)]
    ptb = T(pst("ptb", [128, 1024], BF16), "ptb")
    gp = Rot(pb[0:4])
    gp7 = Rot(pb)

    def mm(out_ap, lhsT, rhs, start, stop, reads, writes, **kw):
        E.op("pe", lambda: nc.tensor.matmul(out_ap, lhsT, rhs, start=start, stop=stop, **kw), reads, writes)

    def act(out_ap, in_ap, func, reads, writes, **kw):
        E.op("act", lambda: nc.scalar.activation(out_ap, in_ap, func, **kw), reads, writes)

    def dve(fn, reads, writes):
        E.op("dve", fn, reads, writes)

    def V(name):
        return VL[name]

    def vcol(name, i):
        o = VL[name] + i
        return vecs[:, o:o + 1]

    def wload(dram_ap, n, reads=(Din,)):
        w = wslots.next()
        E.dma("pool", w[:, 0:n], dram_ap, reads=list(reads), writes=[w])
        return w

    def dvcol(l, s, k, c):
        o = ((l * NS + s) * NDV + k) * 8 + c
        return dvt[:, o:o + 1]

    def dvvec(l, s, k):
        o = ((l * NS + s) * NDV + k) * 8
        return dvt[:, o:o + 8]

    def modvec(l, blk, s):
        o = (l * 6 + blk) * 8 * NS
        return mod[:, o:o + 8 * NS].rearrange("p (m s) -> p m s", s=NS)[:, :, s]

    def modcol(l, blk, s, c):
        o = ((l * 6 + blk) * 8 + c) * NS + s
        return mod[:, o:o + 1]

    A1, G1, A2, G2, GA2, GB2, GAN, GBN = range(8)

    E.dma("sp", vecs[:, :], vecs_d[:, :], reads=[Din], writes=[vecs])
    E.dma("sp", cT[:, :], cT_d[:, :], reads=[Din], writes=[cT])
    E.dma("pool", sel[:, :], sel_d[:, :], reads=[Din], writes=[sel])
    E.dma("sp", ident_f[:, :], ident_d[:, :], reads=[Din], writes=[ident_f])
    for i in range(2):
        E.dma("sp", rw[i][:, :], rw_d[i], reads=[Din], writes=[rw[i]])
    dve(lambda: nc.vector.memset(ones_bf[:, :], 1.0), [], [ones_bf])
    dve(lambda: nc.vector.memset(ones_f[:, :], 1.0), [], [ones_f])
    dve(lambda: nc.vector.tensor_copy(ident_bf[:, :], ident_f[:, :]), [ident_f], [ident_bf])
    act(cond[:, :], cT[:, :], AF.Silu, [cT], [cond])
    cond_bf = sb("cond_bf", [128, 8 * NS], BF16)
    dve(lambda: nc.vector.tensor_copy(cond_bf[:, :], cond[:, :]), [cond], [cond_bf])
    rwb = [sb("rwb%d" % i, [128, 64], BF16) for i in range(2)]
    for i in range(2):
        dve(lambda: nc.vector.tensor_copy(rwb[i][:, :], rw[i][:, :]), [rw[i]], [rwb[i]])
    lgb = sb("lgb", [128, 8], BF16)

    for l in range(n_layers):
        for blk in range(6):
            for m in range(8):
                w = wload(w_ada_d[l, blk * 8 + m], 1024)
                p = gp.next()
                for kc in range(8):
                    mm(p[:, 0:NS], w[:, kc * 128:(kc + 1) * 128], cond_bf[:, kc * NS:(kc + 1) * NS],
                       kc == 0, kc == 7, [w, cond_bf], [p])
                o = ((l * 6 + blk) * 8 + m) * NS
                dve(lambda: nc.vector.tensor_scalar(mod[:, o:o + NS], p[:, 0:NS], vcol("b_ada", l * 48 + blk * 8 + m),
                                                    None, ALU.add), [p, vecs], [mod])
    for l in range(n_layers):
        for s in range(n_seq):
            dve(lambda: nc.vector.tensor_scalar(dvvec(l, s, A1), modvec(l, 1, s), 1.0, None, ALU.add), [mod], [dvt])
            dve(lambda: nc.vector.tensor_scalar(dvvec(l, s, G1), modvec(l, 2, s), 1.0 / ALPHA, None, ALU.mult), [mod], [dvt])
            dve(lambda: nc.vector.tensor_scalar(dvvec(l, s, A2), modvec(l, 4, s), 1.0, None, ALU.add), [mod], [dvt])
            dve(lambda: nc.vector.tensor_scalar(dvvec(l, s, G2), modvec(l, 5, s), 1.0 / ALPHA, None, ALU.mult), [mod], [dvt])
            g1v = vecs[:, V("ln1_g") + l * 8: V("ln1_g") + l * 8 + 8]
            b1v = vecs[:, V("ln1_b") + l * 8: V("ln1_b") + l * 8 + 8]
            dve(lambda: nc.vector.tensor_tensor(dvvec(l, s, GA2), g1v, dvvec(l, s, A2), ALU.mult), [vecs, dvt], [dvt])
            dve(lambda: nc.vector.tensor_tensor(dvvec(l, s, GB2), b1v, dvvec(l, s, A2), ALU.mult), [vecs, dvt], [dvt])
            dve(lambda: nc.vector.tensor_tensor(dvvec(l, s, GB2), dvvec(l, s, GB2), modvec(l, 3, s), ALU.add), [mod, dvt], [dvt])
    for l in range(n_layers - 1):
        for s in range(n_seq):
            g2v = vecs[:, V("ln2_g") + l * 8: V("ln2_g") + l * 8 + 8]
            b2v = vecs[:, V("ln2_b") + l * 8: V("ln2_b") + l * 8 + 8]
            dve(lambda: nc.vector.tensor_tensor(dvvec(l, s, GAN), g2v, dvvec(l + 1, s, A1), ALU.mult), [vecs, dvt], [dvt])
            dve(lambda: nc.vector.tensor_tensor(dvvec(l, s, GBN), b2v, dvvec(l + 1, s, A1), ALU.mult), [vecs, dvt], [dvt])
            dve(lambda: nc.vector.tensor_tensor(dvvec(l, s, GBN), dvvec(l, s, GBN), modvec(l + 1, 0, s), ALU.add), [mod, dvt], [dvt])
    lamv = T(arena[:, 8192:8192 + 2 * L * 256].bitcast(F32), "lamv")
    E.dma("sp", lamv[:, :], lam_d[:, :], reads=[Din], writes=[lamv])
    for l in range(n_layers):
        lam_init = 0.8 - 0.6 * math.exp(-0.3 * l)
        lo = l * 256
        for j in range(2):
            dve(lambda: nc.vector.tensor_tensor(lamtmp[:, 0:64], lamv[:, lo + j * 128: lo + j * 128 + 64],
                                                lamv[:, lo + j * 128 + 64: lo + j * 128 + 128], ALU.mult), [lamv], [lamtmp])
            dve(lambda: nc.vector.reduce_sum(lamtmp[:, 64 + j:65 + j], lamtmp[:, 0:64], axis=AX.X), [lamtmp], [lamtmp])
            act(lamtmp[:, 66 + j:67 + j], lamtmp[:, 64 + j:65 + j], AF.Exp, [lamtmp], [lamtmp])
        dve(lambda: nc.vector.tensor_tensor(lamtmp[:, 64:65], lamtmp[:, 67:68], lamtmp[:, 66:67], ALU.subtract), [lamtmp], [lamtmp])
        dve(lambda: nc.vector.tensor_scalar(nlam[:, l:l + 1], lamtmp[:, 64:65], -lam_init, None, ALU.add), [lamtmp], [nlam])

    arena_prev = [lamv]
    hprev = []

    def layernorm(tb, lng, lnb, l, GAk, GBk, s, write_h):
        psum_s = pb[4]
        psum_q = pb[5]
        for dc in range(8):
            ub = tmpB.next()
            act(ub[:, :], xT[dc][tb][:, :], AF.Copy, [xT[dc][tb]], [ub])
            mm(psum_s[:, :], ones_bf[:, :], ub[:, :], dc == 0, dc == 7, [ones_bf, ub], [psum_s])
            uq = tmpB.next()
            act(uq[:, :], xT[dc][tb][:, :], AF.Square, [xT[dc][tb]], [uq])
            mm(psum_q[:, :], ones_bf[:, :], uq[:, :], dc == 0, dc == 7, [ones_bf, uq], [psum_q])
        mean = tmpA.next()
        dve(lambda: nc.vector.tensor_scalar(mean[:, :], psum_s[:, :], 1.0 / D, None, ALU.mult), [psum_s], [mean])
        msq = tmpA.next()
        dve(lambda: nc.vector.tensor_tensor(msq[:, :], mean[:, :], mean[:, :], ALU.mult), [mean], [msq])
        dve(lambda: nc.vector.scalar_tensor_tensor(msq[:, :], psum_q[:, :], 1.0 / D, msq[:, :], ALU.mult, ALU.subtract),
            [psum_q, msq], [msq])
        act(msq[:, :], msq[:, :], AF.Sqrt, [msq], [msq], bias=LN_EPS / (ALPHA * ALPHA), scale=1.0)
        rstd = tmpA.next()
        dve(lambda: nc.vector.reciprocal(rstd[:, :], msq[:, :]), [msq], [rstd])
        for dc in range(8):
            x_ = xT[dc][tb]
            dve(lambda: nc.vector.tensor_tensor(x_[:, :], x_[:, :], mean[:, :], ALU.subtract), [x_, mean], [x_])
            dve(lambda: nc.vector.tensor_tensor(x_[:, :], x_[:, :], rstd[:, :], ALU.mult), [x_, rstd], [x_])
            if write_h:
                h_ = hT[dc][tb]
                dve(lambda: nc.vector.tensor_scalar(h_[:, :], x_[:, :], dvcol(l, s, GAk, dc), dvcol(l, s, GBk, dc),
                                                    ALU.mult, ALU.add), [x_, dvt], [h_])
            dve(lambda: nc.vector.tensor_scalar(x_[:, :], x_[:, :], vcol(lng, l * 8 + dc), vcol(lnb, l * 8 + dc),
                                                ALU.mult, ALU.add), [x_, vecs], [x_])

    gps = Rot(pb[0:3])
    gps_mla = Rot([pb[0], pb[1], pb[2], pb[5], pb[6]])

    def attention(QT, KT, Vt, krows, scale, nmaps, dv, s, h, finish_fn):
        Vv = Vt[:, 0:16 * dv].rearrange("p (t d) -> p t d", d=dv)
        accO = [pb[3], pb[5]]
        accS = [pb[4], pb[6]]
        sbanks = gps if nmaps == 2 else gps_mla

        def scores(qb, kt):
            qs = slice(qb * 512, (qb + 1) * 512)
            ks = slice(kt * 128, (kt + 1) * 128)
            PTl = []
            if nmaps == 2:
                et = Ets.next()
                E.dma("sp", et[:, :], etab_d[s, h, kt * 128:(kt + 1) * 128, qs], reads=[Tetab[s][h]], writes=[et])
            for m in range(nmaps):
                p = sbanks.next()
                if nmaps == 2:
                    rows = slice(m * 64, (m + 1) * 64)
                else:
                    rows = slice(0, krows)
                mm(p[:, :], KT[rows, ks], QT[rows, qs], True, True, [KT, QT], [p])
                pt = PTs.next()
                act(pt[:, :], p[:, :], AF.Exp, [p], [pt], scale=scale)
                if nmaps == 2:
                    dve(lambda: nc.vector.tensor_tensor(pt[:, :], pt[:, :], et[:, :], ALU.mult), [pt, et], [pt])
                PTl.append(pt)
            return PTl

        def pv(kt, PTl):
            for m in range(nmaps):
                mm(accO[m][0:dv, :], Vv[:, kt, :], PTl[m][:, :], kt == 0, kt == 15, [Vt, PTl[m]], [accO[m]])
                mm(accS[m][0:dv, :], ones_bf[:, 0:dv], PTl[m][:, :], kt == 0, kt == 15, [ones_bf, PTl[m]], [accS[m]])

        for qb in range(4):
            cur = scores(qb, 0)
            for kt in range(16):
                nxt = scores(qb, kt + 1) if kt < 15 else None
                pv(kt, cur)
                cur = nxt
            finish_fn(qb, accO, accS)

    def run_seq(s):
        nonlocal arena_prev
        for c in range(8):
            for tb in range(4):
                E.dma("sp", xT[c][tb][:, :], xT_d[s, c * 128:(c + 1) * 128, tb * 512:(tb + 1) * 512],
                      reads=[Din], writes=[xT[c][tb]])
        rd = inherit(arena_prev)
        posb_i = T(arena[:, 0:4096].bitcast(I32), "posb_i", rd)
        posb = T(arena[:, 4096:8192].bitcast(F32), "posb", rd)
        ang = T(arena[:, 8192:12288].bitcast(F32), "ang", rd)
        ang2 = T(arena[:, 12288:16384].bitcast(F32), "ang2", rd)
        etmp = Rot([T(arena[:, 16384 + i * 2048:16384 + (i + 1) * 2048], "etmp%d" % i, rd) for i in range(2)])
        E.dma("sp", posb_i[:, :], posb_d[s], reads=[Din], writes=[posb_i])
        E.dma("sp", posk_i[:, :], posk_d[s], reads=[Din], writes=[posk_i])
        dve(lambda: nc.vector.tensor_copy(posb[:, :], posb_i[:, :]), [posb_i], [posb])
        dve(lambda: nc.vector.tensor_copy(posk[:, :], posk_i[:, :]), [posk_i], [posk])
        angi = T(arena[:, 20480:20480 + 2048].bitcast(I32), "angi", rd)

        def reduce_angle(dst, src, shift):
            for hf in range(2):
                cs = slice(hf * 1024, (hf + 1) * 1024)
                dve(lambda: nc.vector.tensor_scalar(dst[:, cs], src[:, cs], shift, 1.0 / TWO_PI, ALU.add, ALU.mult), [src], [dst])
                dve(lambda: nc.vector.tensor_copy(angi[:, :], dst[:, cs]), [dst], [angi])
                dve(lambda: nc.vector.tensor_copy(dst[:, cs], angi[:, :]), [angi], [dst])
                dve(lambda: nc.vector.scalar_tensor_tensor(dst[:, cs], dst[:, cs], -TWO_PI, src[:, cs], ALU.mult, ALU.add), [dst, src], [dst])
                dve(lambda: nc.vector.tensor_scalar(dst[:, cs], dst[:, cs], -3.1415925 - shift, 3.1415925 - shift, ALU.max, ALU.min), [dst], [dst])
                dve(lambda: nc.vector.tensor_scalar(dst[:, cs], dst[:, cs], shift, None, ALU.add), [dst], [dst])
        dve(lambda: nc.vector.tensor_scalar(ang[:, :], posb[:, :], vcol("freq", 0), None, ALU.mult), [posb, vecs], [ang])
        reduce_angle(ang2, ang, math.pi / 2)
        act(tabC[:, :], ang2[:, :], AF.Sin, [ang2], [tabC])
        reduce_angle(ang2, ang, 0.0)
        act(ang[:, :], ang2[:, :], AF.Sin, [ang2], [ang])
        dve(lambda: nc.vector.tensor_scalar(tabS[:, :], ang[:, :], vcol("nsgn", 0), None, ALU.mult), [ang, vecs], [tabS])
        dve(lambda: nc.vector.tensor_scalar(posk[:, :], posk[:, :], -1.0, None, ALU.mult), [posk], [posk])
        for kt in range(16):
            act(ang[:, :], posb[:, :], AF.Abs, [posb, posk], [ang], bias=posk[:, kt:kt + 1], scale=1.0)
            for h in range(8):
                slope = 2.0 ** (-(h + 1))
                et = etmp.next()
                act(et[:, :], ang[:, :], AF.Exp, [ang], [et], scale=-slope)
                E.dma("sp", etab_d[s, h, kt * 128:(kt + 1) * 128, :], et[:, :], reads=[et], writes=[Tetab[s][h]])
        arena_prev = [posb_i, posb, ang, ang2, angi] + etmp.items

        for l in range(n_layers):
            lam_init = 0.8 - 0.6 * math.exp(-0.3 * l)
            if stop == 'tables':
                raise Stop()
            if l == 0:
                for c in range(8):
                    for tb in range(4):
                        dve(lambda: nc.vector.tensor_scalar(hT[c][tb][:, :], xT[c][tb][:, :], dvcol(l, s, A1, c),
                                                            modcol(l, 0, s, c), ALU.mult, ALU.add),
                            [xT[c][tb], dvt, mod], [hT[c][tb]])

            if stop == 'p1':
                raise Stop()
            rd = inherit(arena_prev)
            latn = [[T(arena[:, (j * 4 + tb) * 512:(j * 4 + tb + 1) * 512], "latn", rd) for tb in range(4)] for j in range(5)]
            o0 = 5 * 2048
            krT = T(arena[:, o0:o0 + S], "krT", rd)
            o0 += S
            QTm = Rot([T(arena[:, o0 + i * S:o0 + (i + 1) * S], "QTm%d" % i, rd) for i in range(2)])
            o0 += 2 * S
            KTm = Rot([T(arena[:, o0 + i * S:o0 + (i + 1) * S], "KTm%d" % i, rd) for i in range(2)])
            o0 += 2 * S
            Vm = Rot([T(arena[:, o0 + i * 1040:o0 + (i + 1) * 1040], "Vm%d" % i, rd) for i in range(2)])
            o0 += 2 * 1040
            assert o0 <= ARENA_N

            wl = [wload(w_lat_d[l, j], 1024) for j in range(5)]
            for tb in range(4):
                for grp, (j0, j1, n, gname) in enumerate(((0, 3, 384, "qg"), (3, 5, 256, "kvg"))):
                    pq = pb[6]
                    for j in range(j0, j1):
                        p = gp.next()
                        for kc in range(8):
                            mm(p[:, :], wl[j][:, kc * 128:(kc + 1) * 128], hT[kc][tb][:, :], kc == 0, kc == 7,
                               [wl[j], hT[kc][tb]], [p])
                        sq = tmpB.next()
                        dve(lambda: nc.vector.tensor_copy(latn[j][tb][:, :], p[:, :]), [p], [latn[j][tb]])
                        act(sq[:, :], latn[j][tb][:, :], AF.Square, [latn[j][tb]], [sq])
                        mm(pq[:, :], ones_bf[:, :], sq[:, :], j == j0, j == j1 - 1, [ones_bf, sq], [pq])
                    sd = tmpA.next()
                    act(sd[:, :], pq[:, :], AF.Sqrt, [pq], [sd], bias=RMS_EPS, scale=1.0 / n)
                    dve(lambda: nc.vector.reciprocal(sd[:, :], sd[:, :]), [sd], [sd])
                    for j in range(j0, j1):
                        gi = (l * 3 + j) if grp == 0 else (l * 2 + j - 3)
                        dve(lambda: nc.vector.scalar_tensor_tensor(latn[j][tb][:, :], latn[j][tb][:, :], vcol(gname, gi),
                                                                   sd[:, :], ALU.mult, ALU.mult),
                            [latn[j][tb], vecs, sd], [latn[j][tb]])

            if stop == 'lat':
                raise Stop()
            wka = wload(w_kr_d[l, 0], 768)
            wkb = wload(w_kr_d[l, 1], 768)
            for tb in range(4):
                ts_ = slice(tb * 512, (tb + 1) * 512)
                pa = gp.next()
                pb_ = gp.next()
                for kc in range(8):
                    mm(pa[0:96, :], wka[:, kc * 96:(kc + 1) * 96], hT[kc][tb][:, :], kc == 0, kc == 7, [wka, hT[kc][tb]], [pa])
                for kc in range(8):
                    mm(pb_[0:96, :], wkb[:, kc * 96:(kc + 1) * 96], hT[kc][tb][:, :], kc == 0, kc == 7, [wkb, hT[kc][tb]], [pb_])
                t1 = tmpA.next()
                t2 = tmpA.next()
                dve(lambda: nc.vector.tensor_tensor(t1[0:96, :], pa[0:96, :], tabC[0:96, ts_], ALU.mult), [pa, tabC], [t1])
                dve(lambda: nc.vector.tensor_tensor(t2[0:96, :], pb_[0:96, :], tabS[0:96, ts_], ALU.mult), [pb_, tabS], [t2])
                dve(lambda: nc.vector.tensor_tensor(krT[0:96, ts_], t1[0:96, :], t2[0:96, :], ALU.add), [t1, t2], [krT])

            if stop == 'kr':
                raise Stop()
            for h in range(8):
                wqa = wload(w_q_d[l, h, 0], 288)
                wqb = wload(w_q_d[l, h, 1], 288)
                wkn = wload(w_kn_d[l, h], 128)
                wv = wload(w_v_d[l, h], 128)
                QT = QTm.next()
                KT = KTm.next()
                Vt = Vm.next()
                for tb in range(4):
                    ts_ = slice(tb * 512, (tb + 1) * 512)
                    pa = gp.next()
                    pb_ = gp.next()
                    for kc in range(3):
                        mm(pa[0:96, :], wqa[:, kc * 96:(kc + 1) * 96], latn[kc][tb][:, :], kc == 0, kc == 2, [wqa, latn[kc][tb]], [pa])
                    for kc in range(3):
                        mm(pb_[0:96, :], wqb[:, kc * 96:(kc + 1) * 96], latn[kc][tb][:, :], kc == 0, kc == 2, [wqb, latn[kc][tb]], [pb_])
                    t1 = tmpA.next()
                    t2 = tmpA.next()
                    dve(lambda: nc.vector.tensor_tensor(t1[0:96, :], pa[0:96, :], tabC[0:96, ts_], ALU.mult), [pa, tabC], [t1])
                    dve(lambda: nc.vector.tensor_tensor(t2[0:96, :], pb_[0:96, :], tabS[0:96, ts_], ALU.mult), [pb_, tabS], [t2])
                    dve(lambda: nc.vector.tensor_tensor(QT[0:96, ts_], t1[0:96, :], t2[0:96, :], ALU.add), [t1, t2], [QT])
                    pk = gp.next()
                    for kc in range(2):
                        mm(pk[0:64, :], wkn[:, kc * 64:(kc + 1) * 64], latn[3 + kc][tb][:, :], kc == 0, kc == 1,
                           [wkn, latn[3 + kc][tb]], [pk])
                    act(KT[0:64, ts_], pk[0:64, :], AF.Copy, [pk], [KT])
                    dve(lambda: nc.vector.tensor_copy(KT[64:96, ts_], krT[64:96, ts_]), [krT], [KT])
                Vv = Vt[:, 0:1024].rearrange("p (t d) -> p t d", d=64)
                for tg in range(4):
                    pv = gp.next()
                    for ti in range(4):
                        tt = tg * 4 + ti
                        tb, off = tt // 4, (tt % 4) * 128
                        for kc in range(2):
                            mm(pv[:, ti * 64:(ti + 1) * 64], latn[3 + kc][tb][:, off:off + 128], wv[:, kc * 64:(kc + 1) * 64],
                               (kc == 0 and ti == 0), kc == 1, [latn[3 + kc][tb], wv], [pv], skip_group_check=True)
                    dve(lambda: nc.vector.tensor_copy(Vv[:, tg * 4:(tg + 1) * 4, :],
                                                      pv[:, 0:256].rearrange("p (t d) -> p t d", d=64)), [pv], [Vt])

                def fin_mla(qb, accO, accS, h=h):
                    r = tmpA.next()
                    dve(lambda: nc.vector.reciprocal(r[0:64, :], accS[0][0:64, :]), [accS[0]], [r])
                    st = OTst.next()
                    dve(lambda: nc.vector.tensor_tensor(st[0:64, :], accO[0][0:64, :], r[0:64, :], ALU.mult), [accO[0], r], [st])
                    E.dma("sp", otm_d[h * 64:(h + 1) * 64, qb * 512:(qb + 1) * 512], st[0:64, :], reads=[st], writes=[Totm[qb]])

                attention(QT, KT, Vt, 96, MLA_SCALE, 1, 64, s, h, fin_mla)

            if stop == 'mla':
                raise Stop()
            rd = inherit([t for row in latn for t in row] + [krT] + QTm.items + KTm.items + Vm.items)
            o0 = 0
            QTd = Rot([T(arena[:, o0 + i * S:o0 + (i + 1) * S], "QTd%d" % i, rd) for i in range(2)])
            o0 += 2 * S
            KTd = Rot([T(arena[:, o0 + i * S:o0 + (i + 1) * S], "KTd%d" % i, rd) for i in range(2)])
            o0 += 2 * S
            Vd = Rot([T(arena[:, o0 + i * 2064:o0 + (i + 1) * 2064], "Vd%d" % i, rd) for i in range(2)])
            o0 += 2 * 2064
            assert o0 <= ARENA_N
            for h in range(8):
                wdq = wload(w_dq_d[l, h], 1024)
                wdk = wload(w_dk_d[l, h], 1024)
                wdv = wload(w_dv_d[l, h], 1024)
                QT = QTd.next()
                KT = KTd.next()
                Vt = Vd.next()
                for tb in range(4):
                    ts_ = slice(tb * 512, (tb + 1) * 512)
                    for (w_, dst, eng_) in ((wdq, QT, "act"), (wdk, KT, "dve")):
                        p = gp.next()
                        for kc in range(8):
                            mm(p[:, :], w_[:, kc * 128:(kc + 1) * 128], hT[kc][tb][:, :], kc == 0, kc == 7, [w_, hT[kc][tb]], [p])
                        if eng_ == "act":
                            act(dst[:, ts_], p[:, :], AF.Copy, [p], [dst])
                        else:
                            dve(lambda: nc.vector.tensor_copy(dst[:, ts_], p[:, :]), [p], [dst])
                Vv = Vt[:, 0:2048].rearrange("p (t d) -> p t d", d=128)
                for tg in range(4):
                    pv = gp.next()
                    for ti in range(4):
                        tt = tg * 4 + ti
                        tb, off = tt // 4, (tt % 4) * 128
                        for kc in range(8):
                            mm(pv[:, ti * 128:(ti + 1) * 128], hT[kc][tb][:, off:off + 128], wdv[:, kc * 128:(kc + 1) * 128],
                               (kc == 0 and ti == 0), kc == 7, [hT[kc][tb], wdv], [pv], skip_group_check=True)
                    act(Vv[:, tg * 4:(tg + 1) * 4, :], pv[:, :].rearrange("p (t d) -> p t d", d=128), AF.Copy, [pv], [Vt])

                def fin_diff(qb, accO, accS, h=h, l=l, lam_init=lam_init):
                    r0, r1, o0 = tmpA.next(), tmpA.next(), tmpA.next()
                    dve(lambda: nc.vector.reciprocal(r0[:, :], accS[0][:, :]), [accS[0]], [r0])
                    dve(lambda: nc.vector.reciprocal(r1[:, :], accS[1][:, :]), [accS[1]], [r1])
                    dve(lambda: nc.vector.tensor_tensor(o0[:, :], accO[0][:, :], r0[:, :], ALU.mult), [accO[0], r0], [o0])
                    dve(lambda: nc.vector.scalar_tensor_tensor(r1[:, :], accO[1][:, :], nlam[:, l:l + 1], r1[:, :], ALU.mult, ALU.mult),
                        [accO[1], nlam, r1], [r1])
                    dve(lambda: nc.vector.tensor_tensor(o0[:, :], o0[:, :], r1[:, :], ALU.add), [o0, r1], [o0])
                    sq = tmpB.next()
                    act(sq[:, :], o0[:, :], AF.Square, [o0], [sq])
                    pr = gps.next()
                    mm(pr[:, :], ones_bf[:, :], sq[:, :], True, True, [ones_bf, sq], [pr])
                    k2 = (1.0 - lam_init) ** 2
                    act(r0[:, :], pr[:, :], AF.Sqrt, [pr], [r0], bias=RMS_EPS / k2, scale=1.0 / (128.0 * k2))
                    dve(lambda: nc.vector.reciprocal(r0[:, :], r0[:, :]), [r0], [r0])
                    st = OTst.next()
                    dve(lambda: nc.vector.scalar_tensor_tensor(st[:, :], o0[:, :], vcol("dng", l * 8 + h), r0[:, :], ALU.mult, ALU.mult),
                        [o0, vecs, r0], [st])
                    E.dma("sp", otd_d[h * 128:(h + 1) * 128, qb * 512:(qb + 1) * 512], st[:, :], reads=[st], writes=[Totd[qb]])

                attention(QT, KT, Vt, 64, DIFF_SCALE, 2, 128, s, h, fin_diff)

            if stop == 'diff':
                raise Stop()
            rd = inherit(QTd.items + KTd.items + Vd.items)
            otmT = T(arena[:, 0:2048], "otmT", rd)
            otdT = T(arena[:, 2048:6144], "otdT", rd)
            mrg = [T(arena[:, 6144 + c * 512:6144 + (c + 1) * 512], "mrg%d" % c, rd) for c in range(8)]
            is_moe = (l % 2 == 1)
            for tb in range(4):
                ts_ = slice(tb * 512, (tb + 1) * 512)
                for kc in range(4):
                    E.dma("sp", otmT[:, kc * 512:(kc + 1) * 512], otm_d[kc * 128:(kc + 1) * 128, ts_], reads=[Totm[tb]], writes=[otmT])
                for kc in range(8):
                    E.dma("sp", otdT[:, kc * 512:(kc + 1) * 512], otd_d[kc * 128:(kc + 1) * 128, ts_], reads=[Totd[tb]], writes=[otdT])
                for dc in range(8):
                    wbm = wload(w_brm_d[l, dc], 512)
                    wbd = wload(w_brd_d[l, dc], 1024)
                    wga = wload(w_gt_d[l, dc], 1024)
                    wgb = wload(w_gt_d[l, 8 + dc], 1024)
                    pya, pyb, pga, pgb = gp.next(), gp.next(), gp.next(), gp.next()
                    for kc in range(4):
                        mm(pya[:, :], wbm[:, kc * 128:(kc + 1) * 128], otmT[:, kc * 512:(kc + 1) * 512], kc == 0, kc == 3, [wbm, otmT], [pya])
                    for kc in range(8):
                        mm(pyb[:, :], wbd[:, kc * 128:(kc + 1) * 128], otdT[:, kc * 512:(kc + 1) * 512], kc == 0, kc == 7, [wbd, otdT], [pyb])
                    for kc in range(8):
                        mm(pga[:, :], wga[:, kc * 128:(kc + 1) * 128], hT[kc][tb][:, :], kc == 0, kc == 7, [wga, hT[kc][tb]], [pga])
                    for kc in range(8):
                        mm(pgb[:, :], wgb[:, kc * 128:(kc + 1) * 128], hT[kc][tb][:, :], kc == 0, kc == 7, [wgb, hT[kc][tb]], [pgb])
                    sa, sb_ = tmpA.next(), tmpA.next()
                    act(sa[:, :], pga[:, :], AF.Sigmoid, [pga], [sa])
                    act(sb_[:, :], pgb[:, :], AF.Sigmoid, [pgb], [sb_])
                    dve(lambda: nc.vector.tensor_tensor(sa[:, :], sa[:, :], pya[:, :], ALU.mult), [sa, pya], [sa])
                    dve(lambda: nc.vector.tensor_tensor(sb_[:, :], sb_[:, :], pyb[:, :], ALU.mult), [sb_, pyb], [sb_])
                    dve(lambda: nc.vector.tensor_tensor(mrg[dc][:, :], sa[:, :], sb_[:, :], ALU.add), [sa, sb_], [mrg[dc]])
                for dc in range(8):
                    wo = wload(w_out_d[l, dc], 1024)
                    p = gp.next()
                    for kc in range(8):
                        mm(p[:, :], wo[:, kc * 128:(kc + 1) * 128], mrg[kc][:, :], kc == 0, kc == 7, [wo, mrg[kc]], [p])
                    x_ = xT[dc][tb]
                    dve(lambda: nc.vector.scalar_tensor_tensor(x_[:, :], p[:, :], dvcol(l, s, G1, dc), x_[:, :], ALU.mult, ALU.add),
                        [p, dvt, x_], [x_])
                layernorm(tb, "ln1_g", "ln1_b", l, GA2, GB2, s, True)

            if stop == 'merge':
                raise Stop()
            rd = inherit([otmT, otdT] + mrg)
            gTt = T(arena[:, 0:ARENA_N], "gT", rd)
            if is_moe:
                li = l // 2
            nexp = NEXP if is_moe else 1
            for tbb in range(2):
                if is_moe:
                    for t8 in range(8):
                        tb, off = (tbb * 8 + t8) // 4, ((tbb * 8 + t8) % 4) * 128
                        p = gp.next()
                        for kc in range(8):
                            mm(p[:, 0:8], hT[kc][tb][:, off:off + 128], rwb[li][:, kc * 8:(kc + 1) * 8], kc == 0, kc == 7, [hT[kc][tb], rwb[li]], [p])
                        lg = lgt.next()
                        rbv = vecs[:, V("rb") + li * 8: V("rb") + li * 8 + 8]
                        dve(lambda: nc.vector.tensor_tensor(lg[:, 0:8], p[:, 0:8], rbv, ALU.add), [p, vecs], [lg])
                        dve(lambda: nc.vector.reduce_max(lg[:, 32:33], lg[:, 0:8], axis=AX.X), [lg], [lg])
                        dve(lambda: nc.vector.tensor_scalar(lg[:, 8:16], lg[:, 0:8], lg[:, 32:33], None, ALU.is_equal), [lg], [lg])
                        dve(lambda: nc.vector.scalar_tensor_tensor(lg[:, 16:24], lg[:, 8:16], -1e30, lg[:, 0:8], ALU.mult, ALU.add), [lg], [lg])
                        dve(lambda: nc.vector.reduce_max(lg[:, 33:34], lg[:, 16:24], axis=AX.X), [lg], [lg])
                        dve(lambda: nc.vector.tensor_scalar(lg[:, 24:32], lg[:, 16:24], lg[:, 33:34], None, ALU.is_equal), [lg], [lg])
                        dve(lambda: nc.vector.tensor_tensor(lg[:, 34:35], lg[:, 32:33], lg[:, 33:34], ALU.subtract), [lg], [lg])
                        act(lg[:, 35:36], lg[:, 34:35], AF.Sigmoid, [lg], [lg])
                        act(lg[:, 36:37], lg[:, 34:35], AF.Sigmoid, [lg], [lg], scale=-1.0)
                        dve(lambda: nc.vector.tensor_scalar(lg[:, 8:16], lg[:, 8:16], lg[:, 35:36], None, ALU.mult), [lg], [lg])
                        dve(lambda: nc.vector.scalar_tensor_tensor(lg[:, 40:48], lg[:, 24:32], lg[:, 36:37], lg[:, 8:16], ALU.mult, ALU.add), [lg], [lg])
                        dve(lambda: nc.vector.tensor_copy(lgb[:, 0:8], lg[:, 40:48]), [lg], [lgb])
                        E.op("pe", lambda: nc.tensor.transpose(ptb[0:8, 0:128], lgb[:, 0:8], ident_bf[:, :]), [lgb, ident_bf], [ptb])
                        dve(lambda: nc.vector.tensor_copy(GT[0:8, t8 * 128:(t8 + 1) * 128], ptb[0:8, 0:128]), [ptb], [GT])
                for e in range(nexp):
                    ei = eidx(l, e)
                    if is_moe:
                        g_ = gb.next()
                        for half in range(2):
                            p = gp.next()
                            mm(p[:, :], sel[0:8, e * 128:(e + 1) * 128], GT[0:8, half * 512:(half + 1) * 512], True, True, [sel, GT], [p])
                            act(g_[:, half * 512:(half + 1) * 512], p[:, :], AF.Copy, [p], [g_])
                    for fc in range(NFC):
                        w1 = wload(w1_d[ei, fc], 1024)
                        w3 = wload(w3_d[ei, fc], 1024)
                        for half in range(2):
                            tb = tbb * 2 + half
                            pa = gp.next()
                            pb_ = gp.next()
                            for kc in range(8):
                                mm(pa[:, :], w1[:, kc * 128:(kc + 1) * 128], hT[kc][tb][:, :], kc == 0, kc == 7, [w1, hT[kc][tb]], [pa])
                            for kc in range(8):
                                mm(pb_[:, :], w3[:, kc * 128:(kc + 1) * 128], hT[kc][tb][:, :], kc == 0, kc == 7, [w3, hT[kc][tb]], [pb_])
                            sa = tmpB.next()
                            act(sa[:, :], pa[:, :], AF.Silu, [pa], [sa])
                            gsl = gTt[:, fc * 1024 + half * 512: fc * 1024 + (half + 1) * 512]
                            if is_moe:
                                sa2 = tmpB.next()
                                dve(lambda: nc.vector.tensor_tensor(sa2[:, :], sa[:, :], pb_[:, :], ALU.mult), [sa, pb_], [sa2])
                                dve(lambda: nc.vector.tensor_tensor(gsl, sa2[:, :], g_[:, half * 512:(half + 1) * 512], ALU.mult), [sa2, g_], [gTt])
                            else:
                                dve(lambda: nc.vector.tensor_tensor(gsl, sa[:, :], pb_[:, :], ALU.mult), [sa, pb_], [gTt])
                    for dc in range(8):
                        w2h = [wload(w2_d[ei, dc, 0], 1024), wload(w2_d[ei, dc, 1], 1024), wload(w2_d[ei, dc, 2][:, 0:768], 768)]
                        for half in range(2):
                            tb = tbb * 2 + half
                            p = gp.next()
                            for fc in range(NFC):
                                w2 = w2h[fc // 8]
                                mm(p[:, :], w2[:, (fc % 8) * 128:(fc % 8 + 1) * 128], gTt[:, fc * 1024 + half * 512: fc * 1024 + (half + 1) * 512],
                                   fc == 0, fc == NFC - 1, [w2, gTt], [p])
                            x_ = xT[dc][tb]
                            dve(lambda: nc.vector.scalar_tensor_tensor(x_[:, :], p[:, :], dvcol(l, s, G2, dc), x_[:, :], ALU.mult, ALU.add),
                                [p, dvt, x_], [x_])
                for half in range(2):
                    layernorm(tbb * 2 + half, "ln2_g", "ln2_b", l, GAN, GBN, s, l < n_layers - 1)
            arena_prev = [gTt]


    for s in range(n_seq):
        try:
            run_seq(s)
        except Stop:
            pass
        for c in range(8):
            for tb in range(4):
                E.dma("sp", outT_d[s, c * 128:(c + 1) * 128, tb * 512:(tb + 1) * 512], xT[c][tb][:, :],
                      reads=[xT[c][tb]], writes=[Tout[s]])
    E.finish(Tout)
    return nc, em


def tile_w(W, B):
    K, N = W.shape
    KC = K // 128
    NB = N // B
    return np.ascontiguousarray(W.reshape(KC, 128, NB, B).transpose(2, 1, 0, 3)).reshape(NB, 128, KC * B)


def prep_shared(inp):
    f = lambda a: np.asarray(a, dtype=np.float32)
    w_in = f(inp["w_in"])
    o = {}
    o["w_ada"] = np.stack([tile_w(f(inp["w_ada"][l]), 128) for l in range(L)])
    o["w_lat"] = np.stack([tile_w(w_in[l][:, 0:640], 128) for l in range(L)])
    z64 = np.zeros((D, 64), np.float32)
    kr = []
    for l in range(L):
        r = w_in[l][:, 640:672]
        A = np.concatenate([z64, r], axis=1)
        B = np.concatenate([z64, r[:, 16:32], r[:, 0:16]], axis=1)
        kr.append(np.stack([tile_w(A, 96)[0], tile_w(B, 96)[0]]))
    o["w_kr"] = np.stack(kr)
    c0 = 672
    o["w_dq"] = np.stack([tile_w(w_in[l][:, c0:c0 + 1024], 128) for l in range(L)])
    o["w_dk"] = np.stack([tile_w(w_in[l][:, c0 + 1024:c0 + 2048], 128) for l in range(L)])
    o["w_dv"] = np.stack([tile_w(w_in[l][:, c0 + 2048:c0 + 3072], 128) for l in range(L)])
    o["w_gt"] = np.stack([tile_w(w_in[l][:, c0 + 3072:c0 + 5120], 128) for l in range(L)])
    wq = f(inp["w_q_up"])
    qs = []
    for l in range(L):
        hs = []
        for h in range(8):
            Wh = wq[l][:, h * 96:(h + 1) * 96]
            A = Wh
            B = np.concatenate([np.zeros((384, 64), np.float32), Wh[:, 80:96], Wh[:, 64:80]], axis=1)
            hs.append(np.stack([tile_w(A, 96)[0], tile_w(B, 96)[0]]))
        qs.append(np.stack(hs))
    o["w_q"] = np.stack(qs)
    wkv = f(inp["w_kv_up"])
    o["w_kn"] = np.stack([np.stack([tile_w(wkv[l][:, h * 128:h * 128 + 64], 64)[0] for h in range(8)]) for l in range(L)])
    o["w_v"] = np.stack([np.stack([tile_w(wkv[l][:, h * 128 + 64:h * 128 + 128], 64)[0] for h in range(8)]) for l in range(L)])
    o["w_brm"] = np.stack([tile_w(f(inp["w_br_mla"][l]), 128) for l in range(L)])
    o["w_brd"] = np.stack([tile_w(f(inp["w_br_diff"][l]), 128) for l in range(L)])
    o["w_out"] = np.stack([tile_w(f(inp["w_out"][l]), 128) for l in range(L)])
    w1l, w3l, w2l = [], [], []
    fw1, fw3, fw2 = f(inp["ffn_w1"]), f(inp["ffn_w3"]), f(inp["ffn_w2"])
    mw1, mw3, mw2 = f(inp["moe_w1"]), f(inp["moe_w3"]), f(inp["moe_w2"])
    for i in range(2):
        w1l.append(tile_w(fw1[i], 128)); w3l.append(tile_w(fw3[i], 128)); w2l.append(tile_w(fw2[i], 128))
    for i in range(2):
        for e in range(NEXP):
            w1l.append(tile_w(mw1[i, e], 128)); w3l.append(tile_w(mw3[i, e], 128)); w2l.append(tile_w(mw2[i, e], 128))
    o["w1"] = np.stack(w1l)
    o["w3"] = np.stack(w3l)
    w2a = np.stack(w2l)
    w2p = np.zeros((NE, 8, 128, 24 * 128), np.float32)
    w2p[..., :NFC * 128] = w2a
    o["w2"] = np.ascontiguousarray(w2p.reshape(NE, 8, 128, 3, 1024).transpose(0, 1, 3, 2, 4))
    o["ident"] = np.eye(128, dtype=np.float32)
    o["rw"] = np.stack([tile_w(f(inp["router_w"][i]), 8)[0] for i in range(2)])
    vec = np.zeros((128, NV), np.float32)

    def put(name, arr):
        arr = np.asarray(arr, np.float32)
        vec[:, VL[name]:VL[name] + arr.shape[1]] = arr
    put("b_ada", f(inp["b_ada"]).reshape(L, 48, 128).transpose(2, 0, 1).reshape(128, L * 48))
    for nm, key in (("ln1_g", "ln1_g"), ("ln1_b", "ln1_b"), ("ln2_g", "ln2_g"), ("ln2_b", "ln2_b")):
        put(nm, f(inp[key]).reshape(L, 8, 128).transpose(2, 0, 1).reshape(128, L * 8))
    put("qg", f(inp["q_norm_g"]).reshape(L, 3, 128).transpose(2, 0, 1).reshape(128, L * 3))
    put("kvg", f(inp["kv_norm_g"]).reshape(L, 2, 128).transpose(2, 0, 1).reshape(128, L * 2))
    put("dng", f(inp["diff_norm_g"]).reshape(L, 8, 128).transpose(2, 0, 1).reshape(128, L * 8))
    lam = np.stack([f(inp["lambda_q1"]), f(inp["lambda_k1"]), f(inp["lambda_q2"]), f(inp["lambda_k2"])], axis=1)
    o["lam"] = np.ascontiguousarray(np.broadcast_to(lam.reshape(1, L * 256), (128, L * 256)))
    put("rb", np.broadcast_to(f(inp["router_b"]).reshape(1, 16), (128, 16)))
    inv_freq = (np.float32(10000.0) ** (-np.arange(0, 32, 2, dtype=np.float32) / np.float32(32))).astype(np.float32)
    fr = np.zeros((128, 1), np.float32)
    fr[64:80, 0] = inv_freq
    fr[80:96, 0] = inv_freq
    put("freq", fr)
    ns = np.zeros((128, 1), np.float32)
    ns[64:80, 0] = -1.0
    ns[80:96, 0] = 1.0
    put("nsgn", ns)
    o["vecs"] = vec
    sel = np.zeros((8, 8, 128), np.float32)
    for e in range(8):
        sel[e, e, :] = 1.0
    o["sel"] = sel.reshape(8, 1024)
    return o


def prep_core(inp, core):
    x = np.asarray(inp["x"], np.float32)[core * NS:(core + 1) * NS]
    c = np.asarray(inp["c"], np.float32)[core * NS:(core + 1) * NS]
    pos = np.asarray(inp["positions"], np.int32)[core * NS:(core + 1) * NS]
    o = {}
    o["xT"] = np.ascontiguousarray(x.transpose(0, 2, 1))
    o["posb"] = np.ascontiguousarray(np.broadcast_to(pos[:, None, :], (NS, 128, S)))
    o["posk"] = np.ascontiguousarray(pos.reshape(NS, 16, 128).transpose(0, 2, 1))
    o["cT"] = np.ascontiguousarray(c.reshape(NS, 8, 128).transpose(2, 1, 0)).reshape(128, 8 * NS)
    return o


_CACHE = {}


def kernel(**inputs):
    n = 8
    shared = prep_shared(inputs)
    if "nc" not in _CACHE:
        _CACHE["nc"] = build()[0]
    nc = _CACHE["nc"]
    in_maps = []
    for core in range(n):
        m = dict(shared)
        m.update(prep_core(inputs, core))
        in_maps.append(m)
    res = run_bass_kernel_spmd(nc, in_maps, core_ids=list(range(n)))
    outs = [np.asarray(r["outT"]).transpose(0, 2, 1) for r in res.results]
    return np.ascontiguousarray(np.concatenate(outs, axis=0)).astype(np.float32)
```
